# Optimizing a Trainium2 kernel written in Bass

```python
import math
import jax, jax.numpy as jnp
from jax import lax
import numpy as np

D_MODEL = 1024
BATCH = 8
SEQ = 4096
DEPTH = 2

N_A_LAYERS = max(1, DEPTH // 2)
N_B_LAYERS = DEPTH - N_A_LAYERS
SGU_CHUNK = 128
SGU_GROUPS = 8
D_SGU = 3 * D_MODEL
SGU_GROUP_DIM = D_SGU // SGU_GROUPS
HEAD_DIM = 64
N_HEADS = D_MODEL // HEAD_DIM
MOBA_BLOCK = 256
MOBA_TOPK = 3
Q_CHUNK = 128
N_EXPERTS = 32
TOP_K = 4
D_EXPERT = D_MODEL
SWIGLU_LIMIT = 7.0
SWIGLU_ALPHA = 1.702
EXPERT_BLOCK = 256
EPS = 1e-6

kernel_name = 'hybrid_sgu_moba_moe'


def _rmsnorm(x, g):
    xf = x.astype(jnp.float32)
    y = xf * lax.rsqrt(jnp.mean(xf * xf, axis=-1, keepdims=True) + EPS)
    return (y * g.astype(jnp.float32)).astype(x.dtype)


def _modulate(x, g, shift, scale):
    return _rmsnorm(x, g) * (1 + scale[:, None, :]) + shift[:, None, :]


def _alibi_slopes():
    return 2.0 ** (-8.0 * jnp.arange(1, N_HEADS + 1, dtype=jnp.float32) / N_HEADS)


def _chunked_sgu(h, w_in, b_in, v_g, w_s, b_s, w_out):
    B, S, _ = h.shape
    z = jax.nn.gelu(h @ w_in + b_in)
    u, v = jnp.split(z, 2, axis=-1)
    v = _rmsnorm(v, v_g).reshape(B, S // SGU_CHUNK, SGU_CHUNK, SGU_GROUPS, SGU_GROUP_DIM)
    causal = jnp.tril(jnp.ones((SGU_CHUNK, SGU_CHUNK), dtype=bool))
    ws = jnp.where(causal, w_s, jnp.zeros((), w_s.dtype))
    mixed = jnp.einsum('gts,bnsgc->bntgc', ws, v) + b_s.T[:, :, None]
    y = u * mixed.reshape(B, S, D_SGU)
    return y @ w_out


def _shared_kv(x, c, kv_ada_w, kv_ada_b, kv_norm_g, w_kv, k_norm_g):
    B, S, _ = x.shape
    shift, scale = jnp.split(jax.nn.silu(c) @ kv_ada_w + kv_ada_b, 2, axis=-1)
    h = _modulate(x, kv_norm_g, shift, scale)
    k, v = jnp.split(h @ w_kv, 2, axis=-1)
    k = _rmsnorm(k.reshape(B, S, N_HEADS, HEAD_DIM), k_norm_g)
    v = v.reshape(B, S, N_HEADS, HEAD_DIM)
    n_blocks = -(-S // MOBA_BLOCK)
    pad = n_blocks * MOBA_BLOCK - S

    def to_blocks(t):
        t = jnp.pad(t, ((0, 0), (0, pad), (0, 0), (0, 0)))
        return t.reshape(B, n_blocks, MOBA_BLOCK, N_HEADS, HEAD_DIM).transpose(0, 3, 1, 2, 4)

    kb, vb = to_blocks(k), to_blocks(v)
    k_mean = jnp.mean(kb.astype(jnp.float32), axis=3).astype(kb.dtype)
    return kb, vb, k_mean


def _moba_attention(q, kb, vb, k_mean):
    B, S, H, Dh = q.shape
    n_blocks = kb.shape[2]
    n_sel = min(MOBA_TOPK, n_blocks)
    n_chunks = S // Q_CHUNK
    scale = HEAD_DIM ** -0.5
    slopes = _alibi_slopes()
    qh = q.transpose(0, 2, 1, 3)
    head_ix = jnp.arange(H)[:, None, None]
    blk_ar = jnp.arange(n_blocks)
    key_ar = jnp.arange(MOBA_BLOCK)

    def chunk(args):
        b, ci = args
        t0 = ci * Q_CHUNK
        own = t0 // MOBA_BLOCK
        qc = lax.dynamic_slice_in_dim(qh[b], t0, Q_CHUNK, axis=1)
        kbb, vbb = kb[b], vb[b]
        qpos = t0 + jnp.arange(Q_CHUNK)
        gate = jnp.einsum('hqd,hnd->hqn', qc, k_mean[b]).astype(jnp.float32)
        gate = jnp.where(blk_ar < own, gate, -jnp.inf)
        _, sel = lax.top_k(gate, n_sel)
        sel_valid = jnp.arange(n_sel) < own
        ksel = kbb[head_ix, sel]
        vsel = vbb[head_ix, sel]
        s_sel = jnp.einsum('hqd,hqjkd->hqjk', qc, ksel).astype(jnp.float32) * scale
        pos_sel = sel[..., None] * MOBA_BLOCK + key_ar
        s_sel = s_sel - slopes[:, None, None, None] * jnp.abs(qpos[None, :, None, None] - pos_sel).astype(jnp.float32)
        s_sel = jnp.where(sel_valid[None, None, :, None], s_sel, -jnp.inf)
        kown, vown = kbb[:, own], vbb[:, own]
        d_own = qpos[:, None] - (own * MOBA_BLOCK + key_ar)[None, :]
        s_own = jnp.einsum('hqd,hkd->hqk', qc, kown).astype(jnp.float32) * scale
        s_own = s_own - slopes[:, None, None] * jnp.abs(d_own).astype(jnp.float32)
        s_own = jnp.where(d_own >= 0, s_own, -jnp.inf)
        n_k = n_sel * MOBA_BLOCK
        p = jax.nn.softmax(jnp.concatenate([s_sel.reshape(H, Q_CHUNK, n_k), s_own], axis=-1), axis=-1).astype(vb.dtype)
        p_sel = p[..., :n_k].reshape(H, Q_CHUNK, n_sel, MOBA_BLOCK)
        p_own = p[..., n_k:]
        o = jnp.einsum('hqjk,hqjkd->hqd', p_sel, vsel) + jnp.einsum('hqk,hkd->hqd', p_own, vown)
        return o.transpose(1, 0, 2).reshape(Q_CHUNK, H * Dh)

    b_ids = jnp.repeat(jnp.arange(B), n_chunks)
    c_ids = jnp.tile(jnp.arange(n_chunks), B)
    out = lax.map(chunk, (b_ids, c_ids))
    return out.reshape(B, S, H * Dh)


def _moba_mixer(h, w_q, q_g, w_o, kb, vb, k_mean):
    B, S, _ = h.shape
    q = _rmsnorm((h @ w_q).reshape(B, S, N_HEADS, HEAD_DIM), q_g)
    return _moba_attention(q, kb, vb, k_mean) @ w_o


def _moe(h, w_router, b_router, w_gate, b_gate, w_up, b_up, w_down, b_down):
    B, S, D = h.shape
    N = B * S
    xf = h.reshape(N, D)
    logits = (xf @ w_router + b_router).astype(jnp.float32)
    top_val, top_idx = lax.top_k(logits, TOP_K)
    probs = jax.nn.softmax(top_val, axis=-1)
    A = N * TOP_K
    e_flat = top_idx.reshape(A)
    tok_flat = jnp.arange(A) // TOP_K
    p_flat = probs.reshape(A)
    order = jnp.argsort(e_flat, stable=True)
    e_s, tok_s, p_s = e_flat[order], tok_flat[order], p_flat[order]
    counts = jnp.bincount(e_flat, length=N_EXPERTS)
    padded = (counts + EXPERT_BLOCK - 1) // EXPERT_BLOCK * EXPERT_BLOCK
    start = jnp.cumsum(counts) - counts
    pend = jnp.cumsum(padded)
    pstart = pend - padded
    dest = pstart[e_s] + jnp.arange(A) - start[e_s]
    R = A + N_EXPERTS * EXPERT_BLOCK
    n_blk = R // EXPERT_BLOCK
    row_tok = jnp.full((R,), N, dtype=jnp.int32).at[dest].set(tok_s.astype(jnp.int32))
    row_p = jnp.zeros((R,), jnp.float32).at[dest].set(p_s)
    blk_exp = jnp.minimum(jnp.searchsorted(pend, jnp.arange(n_blk) * EXPERT_BLOCK, side='right'), N_EXPERTS - 1)
    xpad = jnp.concatenate([xf, jnp.zeros((1, D), xf.dtype)], axis=0)

    def expert_block(args):
        toks, e = args
        xb = xpad[toks]
        g = jnp.minimum(xb @ w_gate[e] + b_gate[e], SWIGLU_LIMIT)
        u = jnp.clip(xb @ w_up[e] + b_up[e], -SWIGLU_LIMIT, SWIGLU_LIMIT)
        a = g * jax.nn.sigmoid(SWIGLU_ALPHA * g) * (u + 1)
        return a @ w_down[e] + b_down[e]

    y = lax.map(expert_block, (row_tok.reshape(n_blk, EXPERT_BLOCK), blk_exp))
    y = y.reshape(R, D) * row_p[:, None].astype(y.dtype)
    out = jax.ops.segment_sum(y, row_tok, num_segments=N + 1)[:N]
    return out.reshape(B, S, D)


def setup_inputs(seed: int = 0) -> dict:
    key = jax.random.key(seed)
    ks = iter(jax.random.split(key, 40))
    f32 = jnp.float32

    def nrm(shape, s):
        return jax.random.normal(next(ks), shape, f32) * s

    def gain(shape):
        return 1.0 + nrm(shape, 0.02)

    D, HD = D_MODEL, N_HEADS * HEAD_DIM
    return {
        'x': nrm((BATCH, SEQ, D), 1.0),
        'c': nrm((BATCH, D), 1.0),
        'ada_w': nrm((DEPTH, D, 6 * D), 0.5 * D ** -0.5),
        'ada_b': nrm((DEPTH, 6 * D), 0.02),
        'norm1_g': gain((DEPTH, D)),
        'norm2_g': gain((DEPTH, D)),
        'sgu_w_in': nrm((N_A_LAYERS, D, 2 * D_SGU), D ** -0.5),
        'sgu_b_in': nrm((N_A_LAYERS, 2 * D_SGU), 0.02),
        'sgu_v_g': gain((N_A_LAYERS, D_SGU)),
        'sgu_w_s': nrm((N_A_LAYERS, SGU_GROUPS, SGU_CHUNK, SGU_CHUNK), SGU_CHUNK ** -0.5),
        'sgu_b_s': 1.0 + nrm((N_A_LAYERS, SGU_GROUPS, SGU_CHUNK), 0.1),
        'sgu_w_out': nrm((N_A_LAYERS, D_SGU, D), D_SGU ** -0.5),
        'kv_ada_w': nrm((D, 2 * D), 0.5 * D ** -0.5),
        'kv_ada_b': nrm((2 * D,), 0.02),
        'kv_norm_g': gain((D,)),
        'w_kv': nrm((D, 2 * HD), D ** -0.5),
        'k_norm_g': gain((HEAD_DIM,)),
        'attn_w_q': nrm((N_B_LAYERS, D, HD), D ** -0.5),
        'q_norm_g': gain((N_B_LAYERS, HEAD_DIM)),
        'attn_w_o': nrm((N_B_LAYERS, HD, D), HD ** -0.5),
        'moe_w_router': nrm((DEPTH, D, N_EXPERTS), D ** -0.5),
        'moe_b_router': nrm((DEPTH, N_EXPERTS), 0.01),
        'moe_w_gate': nrm((DEPTH, N_EXPERTS, D, D_EXPERT), D ** -0.5),
        'moe_b_gate': nrm((DEPTH, N_EXPERTS, D_EXPERT), 0.02),
        'moe_w_up': nrm((DEPTH, N_EXPERTS, D, D_EXPERT), D ** -0.5),
        'moe_b_up': nrm((DEPTH, N_EXPERTS, D_EXPERT), 0.02),
        'moe_w_down': nrm((DEPTH, N_EXPERTS, D_EXPERT, D), D_EXPERT ** -0.5),
        'moe_b_down': nrm((DEPTH, N_EXPERTS, D), 0.02),
    }


def reference(x, c, ada_w, ada_b, norm1_g, norm2_g, sgu_w_in, sgu_b_in, sgu_v_g, sgu_w_s, sgu_b_s, sgu_w_out,
              kv_ada_w, kv_ada_b, kv_norm_g, w_kv, k_norm_g, attn_w_q, q_norm_g, attn_w_o,
              moe_w_router, moe_b_router, moe_w_gate, moe_b_gate, moe_w_up, moe_b_up, moe_w_down, moe_b_down):
    c_act = jax.nn.silu(c)
    kb = vb = k_mean = None
    for l in range(DEPTH):
        mod = c_act @ ada_w[l] + ada_b[l]
        sh1, sc1, g1, sh2, sc2, g2 = jnp.split(mod, 6, axis=-1)
        if l < N_A_LAYERS:
            h = _modulate(x, norm1_g[l], sh1, sc1)
            m = _chunked_sgu(h, sgu_w_in[l], sgu_b_in[l], sgu_v_g[l], sgu_w_s[l], sgu_b_s[l], sgu_w_out[l])
        else:
            if l == N_A_LAYERS:
                kb, vb, k_mean = _shared_kv(x, c, kv_ada_w, kv_ada_b, kv_norm_g, w_kv, k_norm_g)
            j = l - N_A_LAYERS
            h = _modulate(x, norm1_g[l], sh1, sc1)
            m = _moba_mixer(h, attn_w_q[j], q_norm_g[j], attn_w_o[j], kb, vb, k_mean)
        x = x + g1[:, None, :] * m
        h2 = _modulate(x, norm2_g[l], sh2, sc2)
        x = x + g2[:, None, :] * _moe(h2, moe_w_router[l], moe_b_router[l], moe_w_gate[l], moe_b_gate[l],
                                       moe_w_up[l], moe_b_up[l], moe_w_down[l], moe_b_down[l])
    return x
```

```python
import numpy as np
from contextlib import ExitStack
import concourse.bass as bass
import concourse.mybir as mybir
from concourse.bass_utils import run_bass_kernel_spmd

F32 = mybir.dt.float32
BF = mybir.dt.bfloat16
I32 = mybir.dt.int32
U32 = mybir.dt.uint32
AF = mybir.ActivationFunctionType
ALU = mybir.AluOpType
AX = mybir.AxisListType

D = 1024
NCH = 8
DS = 3072
NE = 32
EPS = 1e-6
BLK = 512
NEG = -30000.0


class Tok:
    __slots__ = ("sem", "cnt", "key", "eng", "opidx")

    def __init__(self, sem, cnt, key, eng=None, opidx=None):
        self.sem = sem
        self.cnt = cnt
        self.key = key
        self.eng = eng
        self.opidx = opidx


class DSem:
    def __init__(self, cx, name):
        self.sem = cx.take_sem()
        self.cnt = 0
        self.key = name


class Eng:
    def __init__(self, cx, name):
        self.cx = cx
        self.name = name
        self.e = getattr(cx.nc, name)
        self.sem = None
        self.n = 0
        self.seen = {}
        self.key = None
        self.opidx = 0
        self.seen_idx = {}
        self.drained = 0

    def new_phase(self, tag):
        if self.sem is None:
            self.key = "p_%s" % self.name
            self.sem = self.cx.take_sem()
            self.n = 0

    def wait(self, *toks):
        for t in toks:
            if t is None:
                continue
            if isinstance(t, (list, tuple)):
                self.wait(*t)
                continue
            if t.eng is not None:
                if t.eng == self.name:
                    if self.name != "tensor" and t.opidx > self.drained:
                        self.e.drain()
                        self.drained = self.opidx
                    continue
                if self.seen_idx.get(t.eng, 0) >= t.opidx:
                    continue
                self.seen_idx[t.eng] = t.opidx
                if self.cx.needed is None:
                    self.cx.rec.add((t.eng, t.opidx))
                else:
                    assert t.cnt is not None, (t.eng, t.opidx)
                self.e.wait_ge(t.sem, t.cnt)
                continue
            if self.seen.get(t.key, 0) >= t.cnt:
                continue
            self.e.wait_ge(t.sem, t.cnt)
            self.seen[t.key] = t.cnt

    def op(self, fn, *args, deps=(), inc=True, **kw):
        self.wait(*deps)
        ins = getattr(self.e, fn)(*args, **kw)
        if not inc:
            return None
        self.opidx += 1
        if self.cx.needed is None or (self.name, self.opidx) in self.cx.needed:
            self.n += 1
            ins.then_inc(self.sem, 1)
            return Tok(self.sem, self.n, self.key, self.name, self.opidx)
        return Tok(self.sem, None, self.key, self.name, self.opidx)

    def dma(self, out, in_, dsem, deps=(), **kw):
        self.wait(*deps)
        ins = self.e.dma_start(out=out, in_=in_, **kw)
        dsem.cnt += 16
        ins.then_inc(dsem.sem, 16)
        return Tok(dsem.sem, dsem.cnt, dsem.key)


class Ring:
    def __init__(self, bufs):
        self.bufs = bufs
        self.free = [[] for _ in bufs]
        self.i = -1

    def next(self):
        self.i = (self.i + 1) % len(self.bufs)
        return self.bufs[self.i], self.free[self.i], self.i

    def release(self, idx, *toks):
        self.free[idx] = [t for t in toks if t is not None]


class Ctx:
    def __init__(self, nc, needed=None):
        self.nc = nc
        self.needed = needed
        self.rec = set()
        self.es = ExitStack()
        self.pe = Eng(self, "tensor")
        self.dve = Eng(self, "vector")
        self.act = Eng(self, "scalar")
        self.pool = Eng(self, "gpsimd")
        self.sp = Eng(self, "sync")
        self.engs = [self.pe, self.dve, self.act, self.pool, self.sp]
        self.ph = None
        self.nsem = 0
        self.nalloc = 0
        self.pool_free = []
        self.pool_used = []
        self.dsems = []

    def breg(self, val):
        if not hasattr(self, "_bregs"):
            self._bregs = {}
        if val not in self._bregs:
            self._bregs[val] = self.nc.gpsimd.to_reg(val)
        return self._bregs[val]

    def uid(self):
        self.nuid = getattr(self, "nuid", 0) + 1
        return self.nuid

    def take_sem(self):
        if self.pool_free:
            h = self.pool_free.pop()
        else:
            self.nalloc += 1
            h = self.es.enter_context(self.nc.semaphore("sem%d" % self.nalloc))
        self.pool_used.append(h)
        return h

    def dsem(self, name):
        self.nsem += 1
        d = DSem(self, "%s_%d" % (name, self.nsem))
        self.dsems.append(d)
        return d

    def begin_phase(self, tag):
        for e in self.engs:
            e.new_phase(tag)
        self.ph = ExitStack()
        self.ph.__enter__()

    def end_phase(self, final_toks=()):
        for e in self.engs:
            e.wait(*final_toks)
        for d in self.dsems:
            if d.cnt > 0:
                self.sp.wait(Tok(d.sem, d.cnt, d.key))
        self.dsems = []
        self.nc.all_engine_barrier()
        self.ph.__exit__(None, None, None)
        self.ph = None

    def sb(self, name, shape, dt):
        return self.ph.enter_context(self.nc.sbuf_tensor("sb_%s_%d" % (name, self.uid()), list(shape), dt))

    def ps(self, name, shape, dt):
        return self.ph.enter_context(self.nc.psum_tensor("ps_%s_%d" % (name, self.uid()), list(shape), dt))

    def gsb(self, name, shape, dt):
        return self.es.enter_context(self.nc.sbuf_tensor("gsb_" + name, list(shape), dt))


def emit_rstd(cx, ssq, rstd, nfeat, deps):
    t1 = cx.dve.op("tensor_scalar", rstd, ssq, 1.0 / nfeat, EPS, ALU.mult, ALU.add, deps=deps)
    t2 = cx.act.op("activation", rstd, rstd, AF.Sqrt, deps=[t1])
    t3 = cx.dve.op("reciprocal", rstd, rstd, deps=[t2])
    return t3


class G:
    pass


def emit_globals(cx, A, nlayers=2):
    nc = cx.nc
    g = G()
    g.ident_bf = cx.gsb("ident_bf", [128, 128], BF)
    g.ident_f = cx.gsb("ident_f", [128, 128], F32)
    g.ones_bf = cx.gsb("ones_bf", [128, 128], BF)
    g.ones_f = cx.gsb("ones_f", [128, 128], F32)
    g.zeros_bf = cx.gsb("zeros_bf", [128, 128], BF)
    g.modc = cx.gsb("modc", [128, 14, NCH], F32)
    g.gb = cx.gsb("gb", [128, 4, D], F32)
    g.ngc = cx.gsb("ngc", [128, 5, NCH], F32)
    g.gsc = cx.gsb("gsc", [128, 5, NCH], F32)
    g.shc = cx.gsb("shc", [128, 5, NCH], F32)

    cx.begin_phase("g")
    ld = cx.dsem("gld")
    t_id1 = cx.sp.dma(g.ident_f[:], A["ident"], ld)
    t_ng = cx.sp.dma(g.ngc[:], A["ngc"], ld)
    ccol = cx.sb("ccol", [128, NCH], F32)
    t_c = cx.sp.dma(ccol[:], A["c_col"], ld)
    allld = Tok(ld.sem, ld.cnt, ld.key)
    t = cx.dve.op("tensor_copy", g.ident_bf[:], g.ident_f[:], deps=[allld])
    cx.dve.op("memset", g.ones_bf[:], 1.0)
    cx.dve.op("memset", g.zeros_bf[:], 0.0)
    t_ones = cx.dve.op("memset", g.ones_f[:], 1.0)
    cact = cx.sb("cact", [128, NCH], F32)
    t_ca = cx.act.op("activation", cact[:], ccol[:], AF.Silu, deps=[allld])

    NV = 2 * 6 * D + 2 * D
    modrow = cx.sb("modrow", [1, NV], F32)
    brow = cx.sb("brow", [1, NV], F32)
    bl = cx.dsem("gbl")
    cx.sp.dma(brow[0:1, 0:6 * D], A["ada_b"][0:1, :], bl)
    cx.sp.dma(brow[0:1, 6 * D:12 * D], A["ada_b"][1:2, :], bl)
    t_b = cx.sp.dma(brow[0:1, 12 * D:NV], A["kv_ada_b"], bl)
    wring = Ring([cx.sb("adaw%d" % i, [128, NCH, 512], F32) for i in range(2)])
    wsem = [cx.dsem("gw%d" % i) for i in range(2)]
    pr = [cx.ps("g_pr%d" % i, [1, 512], F32) for i in range(2)]
    prfree = [None, None]
    srcs = []
    for l in range(2):
        for n in range(12):
            srcs.append((A["ada_w"][l], n, l * 6 * D + n * 512))
    for n in range(4):
        srcs.append((A["kv_ada_w"], n, 12 * D + n * 512))
    last = None
    for bi, (w, n, off) in enumerate(srcs):
        buf, fr, si = wring.next()
        tl = cx.sp.dma(buf[:], w[:, n * 512:(n + 1) * 512].rearrange("(c p) n -> p c n", p=128), wsem[si], deps=fr)
        p = pr[bi % 2]
        tm = None
        for k in range(NCH):
            tm = cx.pe.op("matmul", p[0:1, :], cact[:, k:k + 1], buf[:, k, :], start=(k == 0), stop=(k == NCH - 1),
                          deps=[tl, t_ca, prfree[bi % 2]] if k == 0 else (), inc=(k == NCH - 1))
        wring.release(si, tm)
        te = cx.dve.op("tensor_tensor", modrow[0:1, off:off + 512], p[0:1, :], brow[0:1, off:off + 512], ALU.add,
                       deps=[tm, t_b])
        prfree[bi % 2] = te
        last = te
    pc = cx.ps("g_pc", [128, 14 * NCH], F32)
    vec_off = []
    for l in range(2):
        for j in range(6):
            vec_off.append(l * 6 * D + j * D)
    vec_off += [12 * D, 13 * D]
    tm = None
    for vi, off in enumerate(vec_off):
        for c in range(NCH):
            tm = cx.pe.op("matmul", pc[:, vi * NCH + c: vi * NCH + c + 1], modrow[0:1, off + c * 128: off + (c + 1) * 128],
                          g.ones_f[0:1, 0:1], start=True, stop=True, deps=[last, t_ones] if (vi == 0 and c == 0) else (),
                          inc=(vi == 13 and c == NCH - 1))
    t_mc = cx.dve.op("tensor_copy", g.modc[:].rearrange("p v c -> p (v c)"), pc[:], deps=[tm])
    pb = [cx.ps("g_pb%d" % i, [128, 512], F32) for i in range(2)]
    pbfree = [None, None]
    k = 0
    tl2 = t_mc
    for gi, off in enumerate([2 * D, 5 * D, 6 * D + 2 * D, 6 * D + 5 * D]):
        for hf in range(2):
            tm = cx.pe.op("matmul", pb[k % 2][:], g.ones_f[0:1, :], modrow[0:1, off + hf * 512: off + (hf + 1) * 512],
                          start=True, stop=True, deps=[pbfree[k % 2]])
            pbfree[k % 2] = cx.dve.op("tensor_copy", g.gb[:, gi, hf * 512:(hf + 1) * 512], pb[k % 2][:], deps=[tm])
            tl2 = pbfree[k % 2]
            k += 1
    pairs = [(0, 0, 1), (2, 3, 4), (1, 6, 7), (3, 9, 10), (4, 12, 13)]
    tt = None
    for oi, (gi, shv, scv) in enumerate(pairs):
        t1 = cx.dve.op("scalar_tensor_tensor", g.gsc[:, oi, :], g.modc[:, scv, :], 1.0, g.ngc[:, gi, :], ALU.add, ALU.mult,
                       deps=[t_mc, allld])
        tt = cx.dve.op("tensor_copy", g.shc[:, oi, :], g.modc[:, shv, :], deps=[t_mc])
    cx.end_phase([tt, tl2, t])
    return g


def emit_norm_T(cx, g, xt, oi, hT, pT, work, deps, pT_free):
    junk, ssq, rstd, xn = work["junk"], work["ssq"], work["rstd"], work["xn"]
    t = cx.act.op("activation", junk[:, 0:D], xt, AF.Square, accum_out=ssq[:], deps=deps)
    t = emit_rstd(cx, ssq[:], rstd[:], D, [t])
    t_xn = cx.act.op("activation", xn[:], xt, AF.Copy, scale=rstd[:], deps=[t])
    tm = None
    for c in range(NCH):
        tm = cx.pe.op("transpose", pT[:, c, :], xn[:, c * 128:(c + 1) * 128], g.ident_bf[:],
                      deps=[t_xn, pT_free] if c == 0 else (), inc=(c == NCH - 1))
    te = None
    for c in range(NCH):
        te = cx.dve.op("tensor_scalar", hT[:, c, :], pT[:, c, :], g.gsc[:, oi, c:c + 1], g.shc[:, oi, c:c + 1],
                       ALU.mult, ALU.add, deps=[tm] if c == 0 else ())
    return te, t_xn


def emit_sgu(cx, g, A, S, x_in, x_out):
    nc = cx.nc
    NT = S // 128
    yT_scr = nc.dram_tensor("yT_scr", [128, 24, S], BF, kind="Internal").ap()
    cx.begin_phase("s1")
    w_in = cx.sb("w_in", [128, NCH, 2 * DS], BF)
    b_in = cx.sb("b_in", [1, 2 * DS], BF)
    vgb = cx.sb("vgb", [128, DS], F32)
    wsT = cx.sb("wsT", [128, 8, 128], BF)
    wsTf = cx.sb("wsTf", [128, 8, 128], F32)
    trimask = cx.sb("trimask", [128, 128], F32)
    bsT = cx.sb("bsT", [128, 8], F32)
    wl = cx.dsem("s1w")
    for c in range(NCH):
        cx.pool.dma(w_in[:, c, :], A["sgu_w_in"][c * 128:(c + 1) * 128, :], wl, max_dma_last_dim=4096)
    cx.pool.dma(b_in[:], A["sgu_b_in"], wl, max_dma_last_dim=4096)
    cx.sp.dma(vgb[:], A["sgu_v_g"].partition_broadcast(128), wl)
    cx.sp.dma(wsTf[:], A["sgu_w_sT"], wl)
    cx.sp.dma(trimask[:], A["trimask"], wl)
    cx.sp.dma(bsT[:], A["sgu_b_sT"], wl)
    t_w = Tok(wl.sem, wl.cnt, wl.key)
    t_ws = None
    for gi in range(8):
        t_ws = cx.dve.op("tensor_tensor", wsT[:, gi, :], wsTf[:, gi, :], trimask[:], ALU.mult, deps=[t_w])

    xr = Ring([cx.sb("s1x%d" % i, [128, D], F32) for i in range(2)])
    xsem = [cx.dsem("s1x%d" % i) for i in range(2)]
    work = dict(junk=cx.sb("s1junk", [128, DS], BF), ssq=cx.sb("s1ssq", [128, 1], F32),
                rstd=cx.sb("s1rstd", [128, 1], F32), xn=cx.sb("s1xn", [128, D], BF))
    hT = cx.sb("s1hT", [128, NCH, 128], BF)
    u = cx.sb("s1u", [128, DS], BF)
    v = cx.sb("s1v", [128, DS], BF)
    vn = cx.sb("s1vn", [128, DS], BF)
    y = cx.sb("s1y", [128, DS], BF)
    yT = Ring([cx.sb("s1yT%d" % i, [128, 24, 128], BF) for i in range(1)])
    ysem = [cx.dsem("s1y%d" % i) for i in range(1)]
    ssqv = cx.sb("s1ssqv", [128, 1], F32)
    rstdv = cx.sb("s1rstdv", [128, 1], F32)
    pT = cx.ps("s1pT", [128, NCH, 128], BF)
    pz = [cx.ps("s1pz%d" % i, [128, 512], F32) for i in range(2)]
    pm = [cx.ps("s1pm%d" % i, [128, 512], F32) for i in range(2)]
    pyT = cx.ps("s1pyT", [128, 24, 128], BF)
    pz_free = [None, None]
    pm_free = [None, None]
    pT_free = None
    pyT_free = None
    hT_free = None
    loads = {}

    def load(i):
        buf, fr, si = xr.next()
        loads[i] = (buf, si, cx.sp.dma(buf[:], x_in[i * 128:(i + 1) * 128, :], xsem[si], deps=fr))

    load(0)
    store_toks = []
    for i in range(NT):
        if i + 1 < NT:
            load(i + 1)
        xt, xsi, tl = loads.pop(i)
        te, t_xn = emit_norm_T(cx, g, xt[:], 0, hT, pT, work, [tl, hT_free], pT_free)
        pT_free = te
        xr.release(xsi, t_xn)
        tz_last = None
        t_gl = []
        for n in range(12):
            p = pz[n % 2]
            for k in range(NCH):
                cx.pe.op("matmul", p[:], hT[:, k, :], w_in[:, k, n * 512:(n + 1) * 512], start=(k == 0), stop=False,
                         deps=[te, t_w, pz_free[n % 2]] if k == 0 else (), inc=False)
            tm = cx.pe.op("matmul", p[:], g.ones_bf[0:1, :], b_in[0:1, n * 512:(n + 1) * 512], start=False, stop=True)
            if n < 6:
                tg = cx.act.op("activation", u[:, n * 512:(n + 1) * 512], p[:], AF.Gelu_apprx_tanh, deps=[tm])
            else:
                tg = cx.act.op("activation", v[:, (n - 6) * 512:(n - 5) * 512], p[:], AF.Gelu_apprx_tanh, deps=[tm])
            pz_free[n % 2] = tg
            t_gl.append(tg)
            tz_last = tm
        hT_free = tz_last
        t = cx.act.op("activation", work["junk"][:], v[:], AF.Square, accum_out=ssqv[:], deps=[t_gl[-1]])
        t = emit_rstd(cx, ssqv[:], rstdv[:], DS, [t])
        t_vn = cx.dve.op("scalar_tensor_tensor", vn[:], v[:], rstdv[:], vgb[:], ALU.mult, ALU.mult, deps=[t, t_w])
        t_y = None
        for gi in range(8):
            p = pm[gi % 2]
            tm = cx.pe.op("matmul", p[:, 0:384], wsT[:, gi, :], vn[:, gi * 384:(gi + 1) * 384], start=True, stop=True,
                          deps=[t_vn, t_ws, pm_free[gi % 2]])
            t_y = cx.dve.op("scalar_tensor_tensor", y[:, gi * 384:(gi + 1) * 384], p[:, 0:384], bsT[:, gi:gi + 1],
                            u[:, gi * 384:(gi + 1) * 384], ALU.add, ALU.mult, deps=[tm, t_gl[5]])
            pm_free[gi % 2] = t_y
        tm = None
        for c in range(24):
            tm = cx.pe.op("transpose", pyT[:, c, :], y[:, c * 128:(c + 1) * 128], g.ident_bf[:],
                          deps=[t_y, pyT_free] if c == 0 else (), inc=(c == 23))
        ybuf, yfr, ysi = yT.next()
        te2 = None
        for q in range(3):
            te2 = cx.act.op("activation", ybuf[:, q * 8:(q + 1) * 8, :], pyT[:, q * 8:(q + 1) * 8, :], AF.Copy,
                            deps=[tm] + yfr if q == 0 else ())
        pyT_free = te2
        ts = cx.sp.dma(yT_scr[:, :, i * 128:(i + 1) * 128], ybuf[:], ysem[ysi], deps=[te2])
        yT.release(ysi, ts)
        store_toks.append(ts)
    cx.end_phase(store_toks[-2:])

    cx.begin_phase("s2")
    w_out = cx.sb("w_out", [128, 24, D], BF)
    wl = cx.dsem("s2w")
    for c in range(24):
        cx.pool.dma(w_out[:, c, :], A["sgu_w_out"][c * 128:(c + 1) * 128, :], wl, max_dma_last_dim=4096)
    t_w = Tok(wl.sem, wl.cnt, wl.key)
    xr = Ring([cx.sb("s2x%d" % i, [128, D], F32) for i in range(2)])
    yr = Ring([cx.sb("s2y%d" % i, [128, 24, 128], BF) for i in range(2)])
    lsem = [cx.dsem("s2l%d" % i) for i in range(2)]
    orr = Ring([cx.sb("s2o%d" % i, [128, D], F32) for i in range(2)])
    osem = [cx.dsem("s2o%d" % i) for i in range(2)]
    po = [cx.ps("s2po%d" % i, [128, 512], F32) for i in range(4)]
    po_free = [None] * 4
    loads = {}

    def load2(i):
        xb, xf, si = xr.next()
        yb, yf, _ = yr.next()
        cx.sp.dma(xb[:], x_in[i * 128:(i + 1) * 128, :], lsem[si], deps=xf + yf)
        tl = cx.sp.dma(yb[:], yT_scr[:, :, i * 128:(i + 1) * 128], lsem[si])
        loads[i] = (xb, yb, si, tl)

    load2(0)
    store_toks = []
    for i in range(NT):
        if i + 1 < NT:
            load2(i + 1)
        xb, yb, si, tl = loads.pop(i)
        ob, ofr, osi = orr.next()
        tms = []
        for hf in range(2):
            p = po[(i % 2) * 2 + hf]
            tm = None
            for k in range(24):
                tm = cx.pe.op("matmul", p[:], yb[:, k, :], w_out[:, k, hf * 512:(hf + 1) * 512], start=(k == 0),
                              stop=(k == 23), deps=[tl, t_w, po_free[(i % 2) * 2 + hf]] if k == 0 else (), inc=(k == 23))
            t1 = cx.dve.op("tensor_tensor", ob[:, hf * 512:(hf + 1) * 512], p[:], g.gb[:, 0, hf * 512:(hf + 1) * 512],
                           ALU.mult, deps=[tm] + ofr)
            po_free[(i % 2) * 2 + hf] = t1
            tms.append(tm)
        t2 = cx.dve.op("tensor_tensor", ob[:], ob[:], xb[:], ALU.add, deps=[t1])
        yr.release(si, tms[-1])
        xr.release(si, t2)
        ts = cx.sp.dma(x_out[i * 128:(i + 1) * 128, :], ob[:], osem[osi], deps=[t2])
        orr.release(osi, ts)
        store_toks.append(ts)
    cx.end_phase(store_toks[-2:])


def emit_moe(cx, g, A, S, l, xio, dbg=None, stop=None):
    nc = cx.nc
    NT = S // 128
    NBLK = (S * 4) // BLK + NE
    R = NBLK * BLK
    oi = 1 if l == 0 else 3
    h2_scr = nc.dram_tensor("h2_scr%d" % l, [S, D], BF, kind="Internal").ap()
    rowinfo = nc.dram_tensor("rowinfo%d" % l, [R, 2], F32, kind="Internal").ap()
    yrows = nc.dram_tensor("yrows%d" % l, [R, D], F32, kind="Internal").ap()
    mes = ExitStack()
    mes.__enter__()
    Pd = mes.enter_context(nc.sbuf_tensor("sb_Pd%d" % l, [128, NT, NE], F32))
    didx = mes.enter_context(nc.sbuf_tensor("sb_didx%d" % l, [128, NT, 4], I32))
    idxw = mes.enter_context(nc.sbuf_tensor("sb_idxw%d" % l, [128, NBLK], I32))
    idxw4 = mes.enter_context(nc.sbuf_tensor("sb_idxw4%d" % l, [128, NBLK, 4], I32))

    cx.begin_phase("r%d" % l)
    wr = cx.sb("wr", [128, NCH, NE], F32)
    brb = cx.sb("brb", [128, NE], F32)
    lstrict = cx.sb("lstrict", [128, 128], BF)
    lsf = cx.sb("lsf", [128, 128], F32)
    tokid = cx.sb("tokid", [128, NT], F32)
    pcol = cx.sb("pcol", [128, 1], F32)
    jgrid = cx.sb("jgrid", [128, NBLK, NE], F32)
    gsb2 = cx.sb("gsb2", [128, D], F32)
    shb2 = cx.sb("shb2", [128, D], F32)
    cl = cx.dsem("rc")
    cx.sp.dma(wr[:], A["moe_w_router%d" % l].rearrange("(c p) e -> p c e", p=128), cl)
    cx.sp.dma(brb[:], A["moe_b_router%d" % l].rearrange("o e -> (o e)").partition_broadcast(128), cl)
    cx.sp.dma(lsf[:], A["lstrict"], cl)
    cx.sp.dma(tokid[:], A["tokid"][:, 0:NT], cl)
    cx.sp.dma(pcol[:], A["pcol"], cl)
    cx.sp.dma(jgrid[:], A["jgridb"][:, 0:NBLK, :], cl)
    t_c = Tok(cl.sem, cl.cnt, cl.key)
    t_ls = cx.dve.op("tensor_copy", lstrict[:], lsf[:], deps=[t_c])
    diag = cx.sb("diag", [128, 128], F32)
    pbt = cx.ps("r_pb", [128, 512], F32)
    t_free = None
    t_bc = None
    for which, dst in ((g.gsc, gsb2), (g.shc, shb2)):
        for q in range(2):
            tm = None
            tds = []
            for cc in range(4):
                c = q * 4 + cc
                td = cx.dve.op("tensor_scalar", diag[:], g.ident_f[:], which[:, oi, c:c + 1], None, ALU.mult,
                               deps=[tm, t_c])
                tm = cx.pe.op("matmul", pbt[:, cc * 128:(cc + 1) * 128], g.ones_f[:], diag[:], start=True, stop=True,
                              deps=[td, t_free] if cc == 0 else [td])
            t_free = cx.dve.op("tensor_copy", dst[:, q * 512:(q + 1) * 512], pbt[:], deps=[tm])
            t_bc = t_free

    xr = Ring([cx.sb("rx%d" % i, [128, D], F32) for i in range(2)])
    xsem = [cx.dsem("rx%d" % i) for i in range(2)]
    junk = cx.sb("rjunk", [128, D], BF)
    ssq = cx.sb("rssq", [128, 1], F32)
    rstd = cx.sb("rrstd", [128, 1], F32)
    xn = cx.sb("rxn", [128, D], F32)
    tmp = cx.sb("rtmp", [128, D], F32)
    h2r = Ring([cx.sb("rh2%d" % i, [128, D], BF) for i in range(2)])
    hsem = [cx.dsem("rh%d" % i) for i in range(2)]
    h2Th = cx.sb("rh2Th", [128, NCH, 128], BF)
    h2Tl = cx.sb("rh2Tl", [128, NCH, 128], BF)
    hlo = cx.sb("rhlo", [128, D], BF)
    wrh = cx.sb("rwrh", [128, NCH, NE], BF)
    wrl = cx.sb("rwrl", [128, NCH, NE], BF)
    t_wr = cx.dve.op("tensor_copy", wrh[:], wr[:], deps=[t_c])
    t_wr = cx.dve.op("tensor_tensor", wrl[:], wr[:], wrh[:], ALU.subtract, deps=[t_wr])
    lg = cx.sb("rlg", [128, NE], F32)
    m8 = cx.sb("rm8", [128, 8], F32)
    negmax = cx.sb("rnm", [128, 1], F32)
    ex = cx.sb("rex", [128, NE], F32)
    ssum = cx.sb("rssum", [128, 1], F32)
    maskf = cx.sb("rmaskf", [128, NT, NE], F32)
    maskb = cx.sb("rmaskb", [128, NT, NE], BF)
    rank = cx.sb("rrank", [128, NT, NE], F32)
    pTh = cx.ps("r_pTh", [128, NCH, 128], BF)
    pTl = cx.ps("r_pTl", [128, NCH, 128], BF)
    plg = cx.ps("r_plg", [128, NE], F32)
    prk = cx.ps("r_prk", [128, NE], F32)
    pTf_free = None
    plg_free = None
    prk_free = None
    h2T_free = None
    loads = {}

    def load(i):
        buf, fr, si = xr.next()
        loads[i] = (buf, si, cx.sp.dma(buf[:], xio[i * 128:(i + 1) * 128, :], xsem[si], deps=fr))

    load(0)
    st = []
    for i in range(NT):
        if i + 1 < NT:
            load(i + 1)
        xt, xsi, tl = loads.pop(i)
        t = cx.act.op("activation", junk[:], xt[:], AF.Square, accum_out=ssq[:], deps=[tl])
        t = emit_rstd(cx, ssq[:], rstd[:], D, [t])
        t_xn = cx.act.op("activation", xn[:], xt[:], AF.Copy, scale=rstd[:], deps=[t, h2T_free])
        xr.release(xsi, t_xn)
        hb, hfr, hsi = h2r.next()
        t1 = cx.dve.op("tensor_tensor", tmp[:], xn[:], gsb2[:], ALU.mult, deps=[t_xn, t_bc])
        t1 = cx.dve.op("tensor_tensor", tmp[:], tmp[:], shb2[:], ALU.add, deps=[t1])
        t2 = cx.dve.op("tensor_copy", hb[:], tmp[:], deps=[t1] + hfr)
        t3 = cx.dve.op("tensor_tensor", hlo[:], tmp[:], hb[:], ALU.subtract, deps=[t2, h2T_free])
        ts = cx.sp.dma(h2_scr[i * 128:(i + 1) * 128, :], hb[:], hsem[hsi], deps=[t2])
        st.append(ts)
        tm = None
        for c in range(NCH):
            tm = cx.pe.op("transpose", pTh[:, c, :], hb[:, c * 128:(c + 1) * 128], g.ident_bf[:],
                          deps=[t2, pTf_free] if c == 0 else (), inc=(c == NCH - 1))
        tm2 = None
        for c in range(NCH):
            tm2 = cx.pe.op("transpose", pTl[:, c, :], hlo[:, c * 128:(c + 1) * 128], g.ident_bf[:],
                           deps=[t3] if c == 0 else (), inc=(c == NCH - 1))
        h2r.release(hsi, ts, tm)
        te1 = cx.act.op("activation", h2Th[:], pTh[:], AF.Copy, deps=[tm, h2T_free])
        te = cx.act.op("activation", h2Tl[:], pTl[:], AF.Copy, deps=[tm2])
        pTf_free = te
        k = 0
        for c in range(NCH):
            for (lh, rh) in ((h2Th, wrh), (h2Th, wrl), (h2Tl, wrh)):
                tm = cx.pe.op("matmul", plg[:], lh[:, c, :], rh[:, c, :], start=(k == 0), stop=(k == 3 * NCH - 1),
                              deps=[te, t_wr, plg_free] if k == 0 else (), inc=(k == 3 * NCH - 1))
                k += 1
        h2T_free = tm
        t = cx.dve.op("tensor_tensor", lg[:], plg[:], brb[:], ALU.add, deps=[tm, t_c])
        plg_free = t
        t = cx.dve.op("max", m8[:], lg[:], deps=[t])
        t_m = cx.dve.op("tensor_scalar", maskf[:, i, :], lg[:], m8[:, 3:4], None, ALU.is_ge, deps=[t])
        t_mb = cx.dve.op("tensor_copy", maskb[:, i, :], maskf[:, i, :], deps=[t_m])
        t_n = cx.dve.op("tensor_scalar", negmax[:], m8[:, 0:1], -1.0, None, ALU.mult, deps=[t])
        t_e = cx.act.op("activation", ex[:], lg[:], AF.Exp, bias=negmax[:], deps=[t_n])
        t = cx.dve.op("tensor_tensor", ex[:], ex[:], maskf[:, i, :], ALU.mult, deps=[t_e, t_m])
        t = cx.dve.op("tensor_reduce", ssum[:], ex[:], AX.X, ALU.add, deps=[t])
        t = cx.dve.op("reciprocal", ssum[:], ssum[:], deps=[t])
        t = cx.dve.op("tensor_scalar", Pd[:, i, :], ex[:], ssum[:, 0:1], None, ALU.mult, deps=[t])
        mms = [(lstrict, i)] + [(g.ones_bf, ii) for ii in range(i)]
        tmr = None
        for qi, (lhs, ii) in enumerate(mms):
            lastq = qi == len(mms) - 1
            tmr = cx.pe.op("matmul", prk[:], lhs[:], maskb[:, ii, :], start=(qi == 0), stop=lastq,
                           deps=[t_mb, t_ls, prk_free] if qi == 0 else (), inc=lastq)
        prk_free = cx.dve.op("tensor_copy", rank[:, i, :], prk[:], deps=[tmr])
    for ii in range(NT):
        tm = cx.pe.op("matmul", prk[:], g.ones_bf[:], maskb[:, ii, :], start=(ii == 0), stop=(ii == NT - 1),
                      deps=[prk_free] if ii == 0 else (), inc=(ii == NT - 1))
    cnt = cx.sb("rcnt", [128, NE], F32)
    nb = cx.sb("rnb", [128, NE], F32)
    pend = cx.sb("rpend", [128, NE], F32)
    prow = cx.sb("rprow", [128, NE], F32)
    onesr = cx.sb("rones", [128, NE], F32)
    t = cx.dve.op("tensor_copy", cnt[:], prk[:], deps=[tm])
    t = cx.dve.op("memset", onesr[:], 1.0, deps=[t])
    t = cx.dve.op("tensor_scalar", nb[:], cnt[:], 0.0, None, ALU.is_gt, deps=[t])
    for m in range(1, S // BLK):
        t = cx.dve.op("scalar_tensor_tensor", nb[:], cnt[:], float(m * BLK), nb[:], ALU.is_gt, ALU.add, deps=[t])
    t = cx.dve.op("tensor_tensor_scan", pend[:], onesr[:], nb[:], 0.0, ALU.mult, ALU.add, deps=[t])
    t = cx.dve.op("tensor_tensor", prow[:], pend[:], nb[:], ALU.subtract, deps=[t])
    t = cx.dve.op("tensor_scalar", prow[:], prow[:], float(BLK), None, ALU.mult, deps=[t])
    t = cx.dve.op("tensor_tensor", rank[:], rank[:], prow[:].unsqueeze(1).to_broadcast([128, NT, NE]), ALU.add, deps=[t])
    t = cx.dve.op("scalar_tensor_tensor", rank[:], rank[:], 1.0, maskf[:], ALU.add, ALU.mult, deps=[t])
    t = cx.dve.op("tensor_scalar", rank[:], rank[:], -1.0, None, ALU.add, deps=[t])
    top8 = cx.sb("rtop8", [128, NT, 8], F32)
    for i in range(NT):
        t = cx.dve.op("max", top8[:, i, :], rank[:, i, :], deps=[t] if i == 0 else ())
    t_top = cx.dve.op("tensor_copy", didx[:], top8[:, :, 0:4], deps=[t])
    sc = cx.sb("rsc", [128, NT, 4, 2], F32)
    eq = cx.sb("req", [128, NT, NE], F32)
    tsc = None
    for k in range(4):
        t = cx.dve.op("tensor_tensor", eq[:], rank[:], top8[:, :, k:k + 1].to_broadcast([128, NT, NE]), ALU.is_equal,
                      deps=[t_top])
        t = cx.dve.op("tensor_tensor", eq[:], eq[:], Pd[:], ALU.mult, deps=[t])
        t = cx.dve.op("tensor_reduce", sc[:, :, k, 1], eq[:], AX.X, ALU.add, deps=[t])
        tsc = cx.dve.op("tensor_copy", sc[:, :, k, 0], tokid[:], deps=[t])
    cmp3 = cx.sb("rcmp", [128, NBLK, NE], F32)
    be = cx.sb("rbe", [128, NBLK], F32)
    t = cx.dve.op("tensor_tensor", cmp3[:], pend[:].unsqueeze(1).to_broadcast([128, NBLK, NE]), jgrid[:], ALU.is_le,
                  deps=[tsc])
    t = cx.dve.op("tensor_reduce", be[:], cmp3[:], AX.X, ALU.add, deps=[t])
    unu = cx.sb("runu", [128, NBLK], F32)
    t = cx.dve.op("tensor_scalar", unu[:], be[:], float(NE), 1.0e6, ALU.is_ge, ALU.mult, deps=[t])
    t = cx.dve.op("tensor_scalar", be[:], be[:], float(NE - 1), 128.0, ALU.min, ALU.mult, deps=[t])
    t = cx.dve.op("tensor_scalar", be[:], be[:], pcol[:, 0:1], None, ALU.add, deps=[t])
    be4 = cx.sb("rbe4", [128, NBLK], F32)
    t = cx.dve.op("tensor_tensor", be4[:], be[:], unu[:], ALU.add, deps=[t])
    t_iw = cx.dve.op("tensor_copy", idxw[:], be4[:], deps=[t])
    for h in range(4):
        t = cx.dve.op("tensor_scalar", be4[:], be[:], 4.0, float(h), ALU.mult, ALU.add, deps=[t_iw])
        t = cx.dve.op("tensor_tensor", be4[:], be4[:], unu[:], ALU.add, deps=[t])
        t_iw = cx.dve.op("tensor_copy", idxw4[:, :, h], be4[:], deps=[t])
    zt = cx.sb("rzt", [128, NBLK * 8], F32)
    tz = cx.dve.op("memset", zt[:], 0.0)
    zs = cx.dsem("rz")
    t_z = cx.sp.dma(rowinfo.rearrange("(p a) c -> p (a c)", p=128), zt[:], zs, deps=[tz])
    ss = cx.dsem("rs")
    cx.pool.wait(t_z, tsc, t_top)
    for i in range(NT):
        for k in range(4):
            ins = cx.pool.e.indirect_dma_start(out=rowinfo, out_offset=bass.IndirectOffsetOnAxis(ap=didx[:, i, k:k + 1], axis=0),
                                               in_=sc[:, i, k, :], in_offset=None, bounds_check=cx.breg(R - 1), oob_is_err=False)
            ss.cnt += 16
            ins.then_inc(ss.sem, 16)
    t_sc = Tok(ss.sem, ss.cnt, ss.key)
    dtoks = []
    if dbg is not None:
        ds_ = cx.dsem("dbg")
        cx.sp.wait(t_sc, t_iw)
        dtoks.append(cx.sp.dma(dbg["d_didx"], didx[:], ds_))
        dtoks.append(cx.sp.dma(dbg["d_idxw"], idxw[:], ds_))
        dtoks.append(cx.sp.dma(dbg["d_Pd"], Pd[:], ds_))
        dtoks.append(cx.sp.dma(dbg["d_rowinfo"], rowinfo, ds_))
        dtoks.append(cx.sp.dma(dbg["d_cnt"], cnt[:], ds_))
        dtoks.append(cx.sp.dma(dbg["d_h2"], h2_scr, ds_))
        dtoks = [Tok(ds_.sem, ds_.cnt, ds_.key)]
    cx.end_phase([t_sc, t_iw] + st[-2:] + dtoks)
    if stop == "R":
        mes.__exit__(None, None, None)
        return

    cx.begin_phase("e%d" % l)
    Wg = A["moe_w_gate%d" % l].rearrange("e (p h c) n -> (e p h) (c n)", h=4, c=2)
    Wu = A["moe_w_up%d" % l].rearrange("e (p h c) n -> (e p h) (c n)", h=4, c=2)
    Wd = A["moe_w_down%d" % l].rearrange("e (p h c) n -> (e p h) (c n)", h=4, c=2)
    Bg = A["moe_b_gate%d" % l].rearrange("e (p c) -> (e p) c", c=8)
    Bu = A["moe_b_up%d" % l].rearrange("e (p c) -> (e p) c", c=8)
    wgs = [cx.sb("wg%d" % i, [128, NCH, D], BF) for i in range(2)]
    wus = [cx.sb("wu%d" % i, [128, NCH, D], BF) for i in range(2)]
    wds = [cx.sb("wd%d" % i, [128, NCH, D], BF) for i in range(2)]
    bgs = [cx.sb("bg%d" % i, [128, NCH], F32) for i in range(2)]
    bus = [cx.sb("bu%d" % i, [128, NCH], F32) for i in range(2)]
    wsem = [cx.dsem("ew%d" % i) for i in range(2)]
    w_free = [[], []]
    ris = [cx.sb("ri%d" % i, [128, 4, 2], F32) for i in range(2)]
    tixs = [cx.sb("tix%d" % i, [128, 4], I32) for i in range(2)]
    risem = [cx.dsem("eri%d" % i) for i in range(2)]
    ri_free = [[], []]
    xgs = [cx.sb("xg%d" % i, [128, 4, D], BF) for i in range(2)]
    xgsem = [cx.dsem("exg%d" % i) for i in range(2)]
    xg_free = [[], []]
    xT = cx.sb("exT", [128, NCH, BLK], BF)
    a = cx.sb("ea", [128, NCH, BLK], BF)
    gc = [cx.sb("egc%d" % i, [128, BLK], F32) for i in range(2)]
    sg = [cx.sb("esg%d" % i, [128, BLK], F32) for i in range(2)]
    u1 = [cx.sb("eu1%d" % i, [128, BLK], F32) for i in range(2)]
    ysb = [cx.sb("ey%d" % i, [128, D], F32) for i in range(2)]
    ysem = [cx.dsem("ey%d" % i) for i in range(2)]
    y_free = [[], []]
    ptr = [cx.ps("e_ptr%d" % i, [128, 2 * BLK], BF) for i in range(2)]
    pg = [cx.ps("e_pg%d" % i, [128, BLK], F32) for i in range(2)]
    pu = [cx.ps("e_pu%d" % i, [128, BLK], F32) for i in range(2)]
    pd = [cx.ps("e_pd%d" % i, [128, BLK], F32) for i in range(2)]
    ptr_free = [None, None]
    pg_free = [None, None]
    pu_free = [None, None]
    pd_free = [None, None]
    xT_free = None
    a_free = None
    tmp_free = [None, None]
    pre = {}

    def prefetch(j):
        s_ = j % 2
        cx.pool.wait(w_free[s_])
        toks = []
        for dst, src in ((wgs[s_], Wg), (wus[s_], Wu), (wds[s_], Wd)):
            for h in range(4):
                ins = cx.pool.e.indirect_dma_start(out=dst[:, 2 * h:2 * h + 2, :].rearrange("p c n -> p (c n)"),
                                                   out_offset=None, in_=src,
                                                   in_offset=bass.IndirectOffsetOnAxis(ap=idxw4[:, j, h:h + 1], axis=0),
                                                   bounds_check=cx.breg(NE * 128 * 4 - 1), oob_is_err=False)
                wsem[s_].cnt += 16
                ins.then_inc(wsem[s_].sem, 16)
        for dst, src in ((bgs[s_], Bg), (bus[s_], Bu)):
            ins = cx.pool.e.indirect_dma_start(out=dst[:], out_offset=None, in_=src,
                                               in_offset=bass.IndirectOffsetOnAxis(ap=idxw[:, j:j + 1], axis=0),
                                               bounds_check=cx.breg(NE * 128 - 1), oob_is_err=False)
            wsem[s_].cnt += 16
            ins.then_inc(wsem[s_].sem, 16)
        t_w = Tok(wsem[s_].sem, wsem[s_].cnt, wsem[s_].key)
        t_ri = cx.sp.dma(ris[s_][:], rowinfo[j * BLK:(j + 1) * BLK, :].rearrange("(r p) c -> p r c", p=128), risem[s_],
                         deps=ri_free[s_])
        t_ix = cx.dve.op("tensor_copy", tixs[s_][:], ris[s_][:, :, 0], deps=[t_ri])
        cx.pool.wait(t_ix, xg_free[s_])
        for r in range(4):
            ins = cx.pool.e.indirect_dma_start(out=xgs[s_][:, r, :], out_offset=None, in_=h2_scr,
                                               in_offset=bass.IndirectOffsetOnAxis(ap=tixs[s_][:, r:r + 1], axis=0),
                                               bounds_check=cx.breg(S - 1), oob_is_err=False)
            xgsem[s_].cnt += 16
            ins.then_inc(xgsem[s_].sem, 16)
        t_xg = Tok(xgsem[s_].sem, xgsem[s_].cnt, xgsem[s_].key)
        pre[j] = (t_w, t_ri, t_xg)

    prefetch(0)
    yst = []
    nev = 0
    for j in range(NBLK):
        if j + 1 < NBLK:
            prefetch(j + 1)
        s_ = j % 2
        t_w, t_ri, t_xg = pre.pop(j)
        wg_, wu_, wd_, bg_, bu_, xg_, ri_ = wgs[s_], wus[s_], wds[s_], bgs[s_], bus[s_], xgs[s_], ris[s_]
        tev = None
        for c in range(NCH):
            p = ptr[c % 2]
            tm = None
            for r in range(4):
                tm = cx.pe.op("transpose", p[:, r * 128:(r + 1) * 128],
                              xg_[:, r, :].rearrange("t (p c) -> t c p", c=8)[:, c, :], g.ident_bf[:],
                              deps=[t_xg, ptr_free[c % 2], xT_free] if r == 0 else (), inc=(r == 3))
            if c % 2 == 0:
                tev = cx.act.op("activation", xT[:, c, :], p[:, 0:BLK], AF.Copy, deps=[tm])
            else:
                tev = cx.dve.op("tensor_copy", xT[:, c, :], p[:, 0:BLK], deps=[tm])
            ptr_free[c % 2] = tev
            if c == NCH - 2:
                tev_a = tev
        xg_free[s_] = [tm]
        t_xT = [tev, tev_a]
        t_a = None
        tm_last = None
        for m in range(NCH):
            q = m % 2
            for k in range(NCH):
                cx.pe.op("matmul", pg[q][:], wg_[:, k, :].rearrange("q (p c) -> q c p", c=8)[:, m, :], xT[:, k, :],
                         start=(k == 0), stop=(k == NCH - 1),
                         deps=[t_w, t_xT, pg_free[q], a_free] if k == 0 else (), inc=False)
            for k in range(NCH):
                tm = cx.pe.op("matmul", pu[q][:], wu_[:, k, :].rearrange("q (p c) -> q c p", c=8)[:, m, :], xT[:, k, :],
                              start=(k == 0), stop=(k == NCH - 1), deps=[pu_free[q]] if k == 0 else (),
                              inc=(k == NCH - 1))
            tm_last = tm
            t1 = cx.dve.op("tensor_scalar", gc[q][:], pg[q][:], bg_[:, m:m + 1], 7.0, ALU.add, ALU.min,
                           deps=[tm, tmp_free[q]])
            pg_free[q] = t1
            t2 = cx.act.op("activation", sg[q][:], gc[q][:], AF.Sigmoid, scale=1.702, deps=[t1])
            t3 = cx.dve.op("tensor_scalar", u1[q][:], pu[q][:], bu_[:, m:m + 1], 7.0, ALU.add, ALU.min, deps=[tm])
            pu_free[q] = t3
            t4 = cx.dve.op("tensor_scalar", u1[q][:], u1[q][:], -7.0, 1.0, ALU.max, ALU.add, deps=[t3])
            t5 = cx.pool.op("tensor_tensor", gc[q][:], gc[q][:], sg[q][:], ALU.mult, deps=[t2])
            t_a = cx.dve.op("tensor_tensor", a[:, m, :], gc[q][:], u1[q][:], ALU.mult, deps=[t5, t4, a_free])
            tmp_free[q] = t_a
        xT_free = tm_last
        tm = None
        for r in range(4):
            yb = ysb[r % 2]
            for n in range(2):
                q = n
                for k in range(NCH):
                    tm = cx.pe.op("matmul", pd[q][:], a[:, k, r * 128:(r + 1) * 128], wd_[:, k, n * 512:(n + 1) * 512],
                                  start=(k == 0), stop=(k == NCH - 1),
                                  deps=[t_a, pd_free[q]] if k == 0 else (), inc=(k == NCH - 1))
                te = cx.act.op("activation", yb[:, n * 512:(n + 1) * 512], pd[q][:], AF.Copy, scale=ri_[:, r, 1:2],
                               deps=[tm, t_ri] + y_free[r % 2])
                pd_free[q] = te
            ts = cx.sp.dma(yrows[j * BLK + r * 128: j * BLK + (r + 1) * 128, :], yb[:], ysem[r % 2], deps=[te])
            y_free[r % 2] = [ts]
            yst.append(ts)
        a_free = tm
        w_free[s_] = [tm]
        ri_free[s_] = [te]
    dtoks = []
    if dbg is not None:
        ds_ = cx.dsem("dbg2")
        cx.sp.wait(yst[-2:])
        cx.sp.dma(dbg["d_yrows"], yrows, ds_)
        dtoks = [Tok(ds_.sem, ds_.cnt, ds_.key)]
    cx.end_phase(yst[-2:] + dtoks)
    if stop == "E":
        mes.__exit__(None, None, None)
        return

    cx.begin_phase("u%d" % l)
    bd = cx.sb("ubd", [NE, D], F32)
    cl = cx.dsem("uc")
    t_bd = cx.sp.dma(bd[:], A["moe_b_down%d" % l], cl)
    ygs = [cx.sb("uyg%d" % i, [128, 4, D], F32) for i in range(2)]
    ygsem = [cx.dsem("uyg%d" % i) for i in range(2)]
    yg_free = [[], []]
    xs = [cx.sb("ux%d" % i, [128, D], F32) for i in range(2)]
    xsem = [cx.dsem("ux%d" % i) for i in range(2)]
    x_free = [[], []]
    PdT = cx.sb("uPdT", [NE, 128], BF)
    Pdb = cx.sb("uPdb", [128, NE], BF)
    bdb = cx.sb("ubdb", [NE, D], BF)
    t_bd = cx.dve.op("tensor_copy", bdb[:], bd[:], deps=[t_bd])
    acc = [cx.sb("uacc%d" % i, [128, D], F32) for i in range(2)]
    osem = [cx.dsem("uo%d" % i) for i in range(2)]
    o_free = [[], []]
    pPT = cx.ps("u_pPT", [NE, 128], BF)
    pbias = [cx.ps("u_pb%d" % i, [128, 512], F32) for i in range(2)]
    pPT_free = None
    pb_free = [None, None]
    PdT_free = None
    pre = {}

    def prefetch_u(i):
        s_ = i % 2
        cx.pool.wait(yg_free[s_])
        for k in range(4):
            ins = cx.pool.e.indirect_dma_start(out=ygs[s_][:, k, :], out_offset=None, in_=yrows,
                                               in_offset=bass.IndirectOffsetOnAxis(ap=didx[:, i, k:k + 1], axis=0),
                                               bounds_check=cx.breg(R - 1), oob_is_err=False)
            ygsem[s_].cnt += 16
            ins.then_inc(ygsem[s_].sem, 16)
        t_yg = Tok(ygsem[s_].sem, ygsem[s_].cnt, ygsem[s_].key)
        t_x = cx.sp.dma(xs[s_][:], xio[i * 128:(i + 1) * 128, :], xsem[s_], deps=x_free[s_])
        pre[i] = (t_yg, t_x)

    prefetch_u(0)
    st = []
    for i in range(NT):
        if i + 1 < NT:
            prefetch_u(i + 1)
        s_ = i % 2
        t_yg, t_x = pre.pop(i)
        yg, xt, ac = ygs[s_], xs[s_], acc[s_]
        tpb = cx.dve.op("tensor_copy", Pdb[:], Pd[:, i, :], deps=[pPT_free])
        tm = cx.pe.op("transpose", pPT[:], Pdb[:], g.ident_bf[:], deps=[tpb, pPT_free])
        tc = cx.dve.op("tensor_copy", PdT[:], pPT[:], deps=[tm, PdT_free])
        pPT_free = tc
        tms = []
        for n in range(2):
            tms.append(cx.pe.op("matmul", pbias[n][:], PdT[:], bdb[:, n * 512:(n + 1) * 512], start=True, stop=True,
                                deps=[tc, t_bd, pb_free[n]]))
        PdT_free = tms[-1]
        t1 = cx.dve.op("tensor_tensor", ac[:], yg[:, 0, :], yg[:, 1, :], ALU.add, deps=[t_yg] + o_free[s_])
        t2 = cx.pool.op("tensor_tensor", yg[:, 2, :], yg[:, 2, :], yg[:, 3, :], ALU.add, deps=[t_yg])
        t3 = cx.dve.op("tensor_tensor", ac[:], ac[:], yg[:, 2, :], ALU.add, deps=[t1, t2])
        yg_free[s_] = [t3]
        for n in range(2):
            t3 = cx.dve.op("tensor_tensor", ac[:, n * 512:(n + 1) * 512], ac[:, n * 512:(n + 1) * 512], pbias[n][:], ALU.add,
                           deps=[t3, tms[n]])
            pb_free[n] = t3
        t4 = cx.dve.op("tensor_tensor", ac[:], ac[:], g.gb[:, 1 + 2 * l, :], ALU.mult, deps=[t3])
        t5 = cx.dve.op("tensor_tensor", ac[:], ac[:], xt[:], ALU.add, deps=[t4, t_x])
        x_free[s_] = [t5]
        ts = cx.sp.dma(xio[i * 128:(i + 1) * 128, :], ac[:], osem[s_], deps=[t5])
        o_free[s_] = [ts]
        st.append(ts)
    cx.end_phase(st[-2:])
    mes.__exit__(None, None, None)


def emit_attn(cx, g, A, S, xio, dbg=None, stop=None):
    nc = cx.nc
    NT = S // 128
    NB = S // 256
    NJ = S // 512
    HD = 64
    NH = 16
    kT_scr = nc.dram_tensor("kT_scr", [NH * 64, S], BF, kind="Internal").ap()
    qT_scr = nc.dram_tensor("qT_scr", [NH * 64, S], BF, kind="Internal").ap()
    aT_scr = nc.dram_tensor("aT_scr", [NH * 32, S], BF, kind="Internal").ap()
    aes = ExitStack()
    aes.__enter__()
    vaug = aes.enter_context(nc.sbuf_tensor("sb_vaug", [128, NT, NH, 65], BF))

    cx.begin_phase("q")
    w_kv = cx.sb("w_kv", [128, NCH, 2 * D], BF)
    w_q = cx.sb("w_q", [128, NCH, D], BF)
    kgb = cx.sb("kgb", [128, D], F32)
    qgb = cx.sb("qgb", [128, D], F32)
    qaugc = cx.sb("qaugc", [128, 4, NH, 6], F32)
    wl = cx.dsem("qw")
    for c in range(NCH):
        cx.pool.dma(w_kv[:, c, :], A["w_kv"][c * 128:(c + 1) * 128, :], wl, max_dma_last_dim=4096)
        cx.pool.dma(w_q[:, c, :], A["attn_w_q"][c * 128:(c + 1) * 128, :], wl, max_dma_last_dim=4096)
    cx.sp.dma(kgb[:], A["kg_t"].partition_broadcast(128), wl)
    cx.sp.dma(qgb[:], A["qg_t"].partition_broadcast(128), wl)
    cx.sp.dma(qaugc[:], A["qaug_c"], wl)
    t_w = Tok(wl.sem, wl.cnt, wl.key)
    t_qg = cx.dve.op("tensor_scalar", qgb[:], qgb[:], 0.125, None, ALU.mult, deps=[t_w])
    t_v1 = cx.dve.op("memset", vaug[:], 1.0)

    xr = Ring([cx.sb("qx%d" % i, [128, D], F32) for i in range(2)])
    xsem = [cx.dsem("qx%d" % i) for i in range(2)]
    work = dict(junk=cx.sb("qjunk", [128, D], BF), ssq=cx.sb("qssq", [128, 1], F32),
                rstd=cx.sb("qrstd", [128, 1], F32), xn=cx.sb("qxn", [128, D], BF))
    h1T = cx.sb("qh1T", [128, NCH, 128], BF)
    hkT = cx.sb("qhkT", [128, NCH, 128], BF)
    ssq16 = cx.sb("qssq16", [128, 2, NH], F32)
    tmpn = cx.sb("qtmpn", [128, D], F32)
    sq = tmpn
    kn = cx.sb("qkn", [128, D], BF)
    qn = cx.sb("qqn", [128, D], BF)
    KT4 = cx.sb("qKT4", [128, NCH, 256], BF)
    QT4 = cx.sb("qQT4", [128, NCH, 256], BF)
    AT4 = cx.sb("qAT4", [128, 4, 256], BF)
    QTt = cx.sb("qQTt", [128, NCH, 128], BF)
    ksum = cx.sb("qksum", [128, NCH, NT], F32)
    kmT = cx.sb("qkmT", [128, NCH, 2, NB], BF)
    t_kz = cx.dve.op("memset", kmT[:], 0.0)
    gm = cx.sb("qgm", [128, NH, 16], F32)
    m8h = cx.sb("qm8h", [128, NH, 8], F32)
    augt = cx.sb("qaugt", [128, NH, 32], BF)
    stsem = cx.dsem("qst")
    pT = cx.ps("q_pT", [128, NCH, 128], BF)
    pk = cx.ps("q_pk", [128, D], F32)
    pvq = cx.ps("q_pvq", [128, D], F32)
    ptr = cx.ps("q_ptr", [128, NCH, 128], BF)
    pkm = cx.ps("q_pkm", [128, 16], F32)
    pkm_free = None
    pgt = cx.ps("q_pgt", [128, NH, 16], F32)
    t_az = cx.dve.op("memset", augt[:], 0.0)
    t_gz = cx.dve.op("memset", gm[:], -1e30, deps=[t_az])
    pT_free = pk_free = pvq_free = ptr_free = pat_free = pgt_free = None
    t_km = None
    hT_free = None
    st4_free = []
    loads = {}

    def load(i):
        buf, fr, si = xr.next()
        loads[i] = (buf, si, cx.sp.dma(buf[:], xio[i * 128:(i + 1) * 128, :], xsem[si], deps=fr))

    import os
    NTQ = int(os.environ.get("DBG_NTQ", NT))
    QS = int(os.environ.get("DBG_QS", 99))
    load(0)
    st = []
    for i in range(NTQ):
        if i + 1 < NTQ:
            load(i + 1)
        own = i // 2
        i4 = i % 4
        i2 = i % 2
        xt, xsi, tl = loads.pop(i)
        junk, ssq, rstd, xn = work["junk"], work["ssq"], work["rstd"], work["xn"]
        t = cx.act.op("activation", junk[:], xt[:], AF.Square, accum_out=ssq[:], deps=[tl])
        t = emit_rstd(cx, ssq[:], rstd[:], D, [t])
        t_xn = cx.act.op("activation", xn[:], xt[:], AF.Copy, scale=rstd[:], deps=[t])
        xr.release(xsi, t_xn)
        tm = None
        for c in range(NCH):
            tm = cx.pe.op("transpose", pT[:, c, :], xn[:, c * 128:(c + 1) * 128], g.ident_bf[:],
                          deps=[t_xn, pT_free] if c == 0 else (), inc=(c == NCH - 1))
        te1 = te2 = None
        for c in range(NCH):
            te1 = cx.dve.op("tensor_scalar", h1T[:, c, :], pT[:, c, :], g.gsc[:, 2, c:c + 1], g.shc[:, 2, c:c + 1],
                            ALU.mult, ALU.add, deps=[tm, hT_free] if c == 0 else ())
            te2 = cx.dve.op("tensor_scalar", hkT[:, c, :], pT[:, c, :], g.gsc[:, 4, c:c + 1], g.shc[:, 4, c:c + 1],
                            ALU.mult, ALU.add, deps=[tm, hT_free] if c == 0 else ())
        pT_free = [te1, te2]
        if QS < 1:
            continue
        tmk = None
        for n in range(2):
            for k in range(NCH):
                tmk = cx.pe.op("matmul", pk[:, n * 512:(n + 1) * 512], hkT[:, k, :], w_kv[:, k, n * 512:(n + 1) * 512],
                               start=(k == 0), stop=(k == NCH - 1), deps=[te2, t_w, pk_free] if (k == 0 and n == 0) else (),
                               inc=(k == NCH - 1 and n == 1))
        tmv = None
        for n in range(2):
            for k in range(NCH):
                tmv = cx.pe.op("matmul", pvq[:, n * 512:(n + 1) * 512], hkT[:, k, :],
                               w_kv[:, k, D + n * 512: D + (n + 1) * 512], start=(k == 0), stop=(k == NCH - 1),
                               deps=[pvq_free] if (k == 0 and n == 0) else (), inc=(k == NCH - 1 and n == 1))
        if QS < 2:
            continue
        t = cx.act.op("activation", sq[:], pk[:], AF.Square, deps=[tmk])
        t = cx.dve.op("tensor_reduce", ssq16[:, 0, :], sq[:].rearrange("p (h d) -> p h d", d=HD), AX.X, ALU.add, deps=[t])
        t = emit_rstd(cx, ssq16[:, 0, :], ssq16[:, 0, :], HD, [t])
        t = cx.dve.op("tensor_tensor", tmpn[:].rearrange("p (h d) -> p h d", d=HD), pk[:].rearrange("p (h d) -> p h d", d=HD),
                      ssq16[:, 0, :].unsqueeze(2).to_broadcast([128, NH, HD]), ALU.mult, deps=[t])
        pk_free = t
        t_kn = cx.dve.op("tensor_tensor", kn[:], tmpn[:], kgb[:], ALU.mult, deps=[t, t_w, ptr_free])
        if QS < 3:
            continue
        t_v = cx.dve.op("tensor_copy", vaug[:, i, :, 0:HD], pvq[:].rearrange("p (h d) -> p h d", d=HD),
                        deps=[tmv, t_v1])
        if QS < 4:
            continue
        tmq = None
        for n in range(2):
            for k in range(NCH):
                qv = os.environ.get("DBG_QVAR", "")
                tmq = cx.pe.op("matmul", pvq[:, n * 512:(n + 1) * 512], (hkT if "h" in qv else h1T)[:, k, :],
                               (w_kv if "w" in qv else w_q)[:, k, n * 512:(n + 1) * 512],
                               start=(k == 0), stop=(k == NCH - 1), deps=[te1, t_v] if (k == 0 and n == 0) else (),
                               inc=(k == NCH - 1 and n == 1))
        hT_free = tmq
        if QS < 5:
            continue
        tmt = None
        for h in range(NCH):
            tmt = cx.pe.op("transpose", ptr[:, h, :], kn[:, h * 128:(h + 1) * 128], g.ident_bf[:],
                           deps=[t_kn, ptr_free] if h == 0 else (), inc=(h == NCH - 1))
        K5 = int(os.environ.get("DBG_K5", 3))
        t_k4 = t_ks = None
        if K5 & 1:
            t_k4 = cx.act.op("activation", KT4[:, :, i2 * 128:(i2 + 1) * 128], ptr[:], AF.Copy, deps=[tmt] + st4_free)
        if K5 & 2:
            tmk2 = None
            for c in range(NCH):
                tmk2 = cx.pe.op("matmul", pkm[:, c:c + 1], kn[:, c * 128:(c + 1) * 128], g.ones_bf[:, 0:1], start=True, stop=True,
                                deps=[pkm_free] if c == 0 else (), inc=(c == NCH - 1))
            t_ks = cx.dve.op("tensor_copy", ksum[:, :, i], pkm[:, 0:NCH], deps=[tmk2])
            pkm_free = t_ks
        ptr_free = [t_k4, t_ks]
        if K5 != 3:
            continue
        if i % 2 == 1:
            t = cx.dve.op("tensor_tensor", ksum[:, :, i], ksum[:, :, i], ksum[:, :, i - 1], ALU.add, deps=[t_ks])
            cx.dve.op("tensor_scalar", kmT[0:64, :, 0, own], ksum[0:64, :, i], 1.0 / 256.0, None, ALU.mult, deps=[t, t_kz])
            t_km = cx.dve.op("tensor_scalar", kmT[64:128, :, 1, own], ksum[64:128, :, i], 1.0 / 256.0, None, ALU.mult)
        if QS < 6:
            continue
        t = cx.act.op("activation", sq[:], pvq[:], AF.Square, deps=[tmq])
        t = cx.dve.op("tensor_reduce", ssq16[:, 1, :], sq[:].rearrange("p (h d) -> p h d", d=HD), AX.X, ALU.add, deps=[t])
        t = emit_rstd(cx, ssq16[:, 1, :], ssq16[:, 1, :], HD, [t])
        t = cx.dve.op("tensor_tensor", tmpn[:].rearrange("p (h d) -> p h d", d=HD), pvq[:].rearrange("p (h d) -> p h d", d=HD),
                      ssq16[:, 1, :].unsqueeze(2).to_broadcast([128, NH, HD]), ALU.mult, deps=[t])
        pvq_free = t
        t_qn = cx.dve.op("tensor_tensor", qn[:], tmpn[:], qgb[:], ALU.mult, deps=[t, t_qg])
        K6 = int(os.environ.get("DBG_K6", 9))
        if K6 < 2:
            continue
        tmt = None
        for h in range(NCH):
            tmt = cx.pe.op("transpose", ptr[:, h, :], qn[:, h * 128:(h + 1) * 128], g.ident_bf[:],
                           deps=[t_qn, ptr_free] if h == 0 else (), inc=(h == NCH - 1))
        t_q4 = cx.act.op("activation", QT4[:, :, i2 * 128:(i2 + 1) * 128], ptr[:], AF.Copy, deps=[tmt] + st4_free)
        if K6 < 3:
            ptr_free = [t_q4]
            continue
        t_qt = t_q4
        ptr_free = [t_q4, t_qt]
        if QS < 7:
            continue
        if own > 0:
            tmg = None
            for c in range(NCH):
                tmg = cx.pe.op("matmul", pgt[:, 2 * c:2 * c + 2, 0:own], QT4[:, c, i2 * 128:(i2 + 1) * 128], kmT[:, c, :, 0:own],
                               start=True, stop=True,
                               deps=[t_qt, t_km, pgt_free] if c == 0 else (), inc=(c == NCH - 1))
            t = cx.dve.op("tensor_copy", gm[:, :, 0:own], pgt[:, :, 0:own], deps=[tmg, t_gz])
            pgt_free = t
            for h in range(NH):
                t = cx.dve.op("max", m8h[:, h, :], gm[:, h, :], deps=[t] if h == 0 else ())
            for h in range(NH):
                t = cx.dve.op("tensor_scalar", augt[:, h, 0:16], gm[:, h, :], m8h[:, h, 2:3], NEG, ALU.is_lt, ALU.mult,
                              deps=[t, pat_free] if h == 0 else ())
            t = cx.dve.op("memset", augt[:, :, own:own + 1], 0.0, deps=[t])
        else:
            t = None
        t_au = cx.dve.op("tensor_copy", augt[:, :, 16:22], qaugc[:, i4, :, :], deps=[t, t_w, pat_free, t_az])
        tma = None
        for h in range(4):
            tma = cx.pe.op("transpose", ptr[:, h, :], augt[:, 4 * h:4 * h + 4, :].rearrange("p a b -> p (a b)"), g.ident_bf[:],
                           deps=[t_au, ptr_free] if h == 0 else (), inc=(h == 3))
        t_a4 = cx.act.op("activation", AT4[:, :, i2 * 128:(i2 + 1) * 128], ptr[:, 0:4, :], AF.Copy, deps=[tma] + st4_free)
        pat_free = t_a4
        ptr_free = [t_a4]
        if QS < 8:
            continue
        if i2 == 1:
            j0 = (i - 1) * 128
            cx.sp.dma(kT_scr.rearrange("(c p) s -> p c s", p=128)[:, :, j0:j0 + 256], KT4[:], stsem, deps=[t_k4, t_q4, t_a4])
            cx.sp.dma(qT_scr.rearrange("(c p) s -> p c s", p=128)[:, :, j0:j0 + 256], QT4[:], stsem)
            ts = cx.sp.dma(aT_scr.rearrange("(c p) s -> p c s", p=128)[:, :, j0:j0 + 256], AT4[:], stsem)
            st4_free = [ts]
            st.append(ts)
    dtoks = []
    if dbg is not None and QS >= 99:
        ds_ = cx.dsem("dbgq")
        cx.sp.wait(st[-1:])
        cx.sp.dma(dbg["d_kT"], kT_scr, ds_)
        cx.sp.dma(dbg["d_qT"], qT_scr, ds_)
        cx.sp.dma(dbg["d_aT"], aT_scr, ds_)
        cx.sp.dma(dbg["d_v"], vaug[:], ds_, deps=[t_v])
        dtoks = [Tok(ds_.sem, ds_.cnt, ds_.key)]
    cx.end_phase((st[-1:] + [t_v] + dtoks) if QS >= 99 else [])
    if stop == "Q":
        aes.__exit__(None, None, None)
        return

    cx.begin_phase("a")
    cmask = cx.sb("cmask", [128, 4, 512], BF)
    cbias = cx.sb("cbias", [128, NH, 32], F32)
    otok = cx.sb("otok", [128, NT, D], BF)
    wl = cx.dsem("aw")
    cx.pool.dma(cmask[:], A["cmask"], wl, max_dma_last_dim=4096)
    cx.sp.dma(cbias[:], A["cbias"], wl)
    t_w = Tok(wl.sem, wl.cnt, wl.key)
    hsem = [cx.dsem("ah%d" % i) for i in range(2)]
    h_free = [[], []]
    PTs = [cx.sb("aPT%d" % i, [128, 512], BF) for i in range(3)]
    PT_free = [None] * 3
    OTs = cx.sb("aOT", [65, 512], BF)
    Sd = [cx.sb("aSd%d" % i, [128, 512], F32) for i in range(2)]
    Sd_free = [None, None]
    ndiag = 0
    rec = cx.sb("arec", [128, 1], F32)
    sub = ExitStack()
    KTs = [sub.enter_context(nc.sbuf_tensor("sb_aKT%d" % i, [96, S], BF)) for i in range(2)]
    QTs = [sub.enter_context(nc.sbuf_tensor("sb_aQT%d" % i, [96, S], BF)) for i in range(2)]
    pS = [cx.ps("a_pS%d" % i, [128, 512], F32) for i in range(2)]
    pO = [cx.ps("a_pO%d" % i, [65, 512], F32) for i in range(2)]
    pOt = [cx.ps("a_pOt%d" % i, [128, 128], BF) for i in range(2)]
    pS_free = [None, None]
    pO_free = [None, None]
    pOt_free = [None, None]
    OT_free = None
    pre = {}

    def prefetch_h(h):
        s_ = h % 2
        cx.sp.dma(KTs[s_][0:64, :], kT_scr[h * 64:(h + 1) * 64, :], hsem[s_], deps=h_free[s_])
        cx.sp.dma(QTs[s_][64:96, :], aT_scr[h * 32:(h + 1) * 32, :], hsem[s_])
        cx.pool.dma(KTs[s_][64:96, :], A["kaug_c"][h], hsem[s_], deps=h_free[s_], max_dma_last_dim=4096)
        pre[h] = cx.sp.dma(QTs[s_][0:64, :], qT_scr[h * 64:(h + 1) * 64, :], hsem[s_])

    prefetch_h(0)
    npair = 0
    nj = 0
    t_last = None
    import os
    NHL = int(os.environ.get("DBG_NH", NH))
    NJL = int(os.environ.get("DBG_NJ", NJ))
    for h in range(NHL):
        if h + 1 < NHL:
            prefetch_h(h + 1)
        s_ = h % 2
        KT, QT = KTs[s_], QTs[s_]
        t_h = pre.pop(h)
        tmo = None
        for j in range(NJL):
            po = pO[nj % 2]
            ni = 4 * j + 4
            for i in range(ni):
                ps_ = pS[npair % 2]
                PT = PTs[npair % 3]
                tms = cx.pe.op("matmul", ps_[:], KT[:, i * 128:(i + 1) * 128], QT[:, j * 512:(j + 1) * 512], start=True,
                               stop=True, deps=[t_h, pS_free[npair % 2]])
                dd = 4 * j - i + 3
                if i >= 4 * j:
                    sd = Sd[ndiag % 2]
                    td = cx.dve.op("tensor_tensor", sd[:], ps_[:], cmask[:, i - 4 * j, :], ALU.add,
                                   deps=[tms, t_w, Sd_free[ndiag % 2]])
                    pS_free[npair % 2] = td
                    te = cx.act.op("activation", PT[:], sd[:], AF.Exp, bias=cbias[:, h, dd:dd + 1],
                                   deps=[td, PT_free[npair % 3]])
                    Sd_free[ndiag % 2] = te
                    ndiag += 1
                else:
                    te = cx.act.op("activation", PT[:], ps_[:], AF.Exp, bias=cbias[:, h, dd:dd + 1],
                                   deps=[tms, t_w, PT_free[npair % 3]])
                    pS_free[npair % 2] = te
                tmo = cx.pe.op("matmul", po[:], vaug[:, i, h, :], PT[:], start=(i == 0), stop=(i == ni - 1),
                               deps=[te, pO_free[nj % 2]] if i == 0 else [te])
                PT_free[npair % 3] = tmo
                npair += 1
            t_ot = cx.dve.op("tensor_copy", OTs[:], po[:], deps=[tmo, OT_free])
            pO_free[nj % 2] = t_ot
            tmt = None
            for r in range(4):
                pt_ = pOt[r % 2]
                tmt = cx.pe.op("transpose", pt_[:, 0:65], OTs[0:65, r * 128:(r + 1) * 128], g.ident_bf[0:65, 0:65],
                               deps=[t_ot, pOt_free[r % 2]])
                t1 = cx.dve.op("reciprocal", rec[:], pt_[:, 64:65], deps=[tmt])
                t_last = cx.dve.op("tensor_scalar", otok[:, 4 * j + r, h * HD:(h + 1) * HD], pt_[:, 0:64], rec[:, 0:1], None,
                                   ALU.mult, deps=[t1])
                pOt_free[r % 2] = t_last
            OT_free = tmt
            nj += 1
        h_free[s_] = [tmo]
    for e_ in cx.engs:
        e_.wait(tmo, t_last)
    sub.close()
    w_o = cx.sb("w_o", [128, NCH, D], BF)
    wl2 = cx.dsem("aw2")
    for c in range(NCH):
        cx.pool.dma(w_o[:, c, :], A["attn_w_o"][c * 128:(c + 1) * 128, :], wl2, max_dma_last_dim=4096)
    t_w = Tok(wl2.sem, wl2.cnt, wl2.key)
    xr = [cx.sb("ax%d" % i, [128, D], F32) for i in range(2)]
    xsem = [cx.dsem("ax%d" % i) for i in range(2)]
    x_free = [[], []]
    oT = cx.sb("aoT", [128, NCH, 128], BF)
    ob = [cx.sb("aob%d" % i, [128, D], F32) for i in range(2)]
    osem = [cx.dsem("ao%d" % i) for i in range(2)]
    o_free = [[], []]
    poT = cx.ps("a_poT", [128, NCH, 128], BF)
    oT_free = None
    poT_free = None
    pre = {}

    def load_x(i):
        s_ = i % 2
        pre[i] = cx.sp.dma(xr[s_][:], xio[i * 128:(i + 1) * 128, :], xsem[s_], deps=x_free[s_])

    load_x(0)
    st = []
    for i in range(NT):
        if i + 1 < NT:
            load_x(i + 1)
        s_ = i % 2
        t_x = pre.pop(i)
        tm = None
        for c in range(NCH):
            tm = cx.pe.op("transpose", poT[:, c, :], otok[:, i, c * 128:(c + 1) * 128], g.ident_bf[:],
                          deps=[t_last, poT_free] if c == 0 else (), inc=(c == NCH - 1))
        te = cx.act.op("activation", oT[:], poT[:], AF.Copy, deps=[tm, oT_free])
        poT_free = te
        t1 = None
        for n in range(2):
            p = pS[n]
            for k in range(NCH):
                tm = cx.pe.op("matmul", p[:], oT[:, k, :], w_o[:, k, n * 512:(n + 1) * 512], start=(k == 0), stop=(k == NCH - 1),
                              deps=[te, t_w, pS_free[n]] if k == 0 else (), inc=(k == NCH - 1))
            t1 = cx.dve.op("tensor_tensor", ob[s_][:, n * 512:(n + 1) * 512], p[:], g.gb[:, 2, n * 512:(n + 1) * 512], ALU.mult,
                           deps=[tm] + o_free[s_])
            pS_free[n] = t1
        oT_free = tm
        t2 = cx.dve.op("tensor_tensor", ob[s_][:], ob[s_][:], xr[s_][:], ALU.add, deps=[t1, t_x])
        x_free[s_] = [t2]
        ts = cx.sp.dma(xio[i * 128:(i + 1) * 128, :], ob[s_][:], osem[s_], deps=[t2])
        o_free[s_] = [ts]
        st.append(ts)
    cx.end_phase(st[-2:])
    aes.__exit__(None, None, None)


def build_program(S, phases=("sgu",), dbg=False, stop=None):
    _, _, rec = _build_program(S, phases, dbg, stop, None)
    nc, names, _ = _build_program(S, phases, dbg, stop, rec)
    return nc, names


def _build_program(S, phases, dbg, stop, needed):
    nc = bass.Bass("TRN2", target_bir_lowering=False)
    A = {}

    def inp(name, shape, dt=F32):
        A[name] = nc.dram_tensor(name, list(shape), dt, kind="ExternalInput").ap()

    inp("x", [S, D])
    inp("c_col", [128, NCH])
    inp("ident", [128, 128])
    inp("ngc", [128, 5, NCH])
    inp("ada_w", [2, D, 6 * D])
    inp("ada_b", [2, 6 * D])
    inp("kv_ada_w", [D, 2 * D])
    inp("kv_ada_b", [1, 2 * D])
    inp("sgu_w_in", [D, 2 * DS])
    inp("sgu_b_in", [1, 2 * DS])
    inp("sgu_v_g", [DS])
    inp("sgu_w_sT", [128, 8, 128])
    inp("trimask", [128, 128])
    inp("sgu_b_sT", [128, 8])
    inp("sgu_w_out", [DS, D])
    for l_ in range(2):
        if ("moe%d" % l_) not in phases:
            continue
        inp("moe_w_router%d" % l_, [D, NE])
        inp("moe_b_router%d" % l_, [1, NE])
        inp("moe_w_gate%d" % l_, [NE, D, D])
        inp("moe_w_up%d" % l_, [NE, D, D])
        inp("moe_w_down%d" % l_, [NE, D, D])
        inp("moe_b_gate%d" % l_, [NE, D])
        inp("moe_b_up%d" % l_, [NE, D])
        inp("moe_b_down%d" % l_, [NE, D])
    if "attn" in phases:
        inp("w_kv", [D, 2 * D])
        inp("attn_w_q", [D, D])
        inp("attn_w_o", [D, D])
        inp("kg_t", [D])
        inp("qg_t", [D])
        inp("qaug_c", [128, 4, 16, 6])
        inp("kaug_c", [16, 32, S])
        inp("cmask", [128, 4, 512])
        inp("cbias", [128, 16, 32])
    inp("lstrict", [128, 128])
    inp("tokid", [128, 32])
    inp("pcol", [128, 1])
    inp("jgridb", [128, 64, NE])
    y = nc.dram_tensor("y", [S, D], F32, kind="ExternalOutput").ap()
    cx = Ctx(nc, needed)
    with cx.es:
        g = emit_globals(cx, A)
        if "sgu" in phases:
            emit_sgu(cx, g, A, S, A["x"], y)
        if "moe0" in phases:
            dd = None
            if dbg:
                NT_ = S // 128
                NB_ = (S * 4) // BLK + NE
                dd = {}
                for nm, shp, dt in (("d_didx", [128, NT_, 4], I32), ("d_idxw", [128, NB_], I32), ("d_Pd", [128, NT_, NE], F32),
                                    ("d_rowinfo", [NB_ * BLK, 2], F32), ("d_cnt", [128, NE], F32), ("d_yrows", [NB_ * BLK, D], F32), ("d_h2", [S, D], BF)):
                    dd[nm] = nc.dram_tensor(nm, shp, dt, kind="ExternalOutput").ap()
            emit_moe(cx, g, A, S, 0, y, dbg=dd, stop=stop)
        if "copy" in phases:
            cs_ = cx.dsem("cpy")
            tcp = None
            for i_ in range(S // 128):
                tcp = cx.sp.dma(y[i_ * 128:(i_ + 1) * 128, :], A["x"][i_ * 128:(i_ + 1) * 128, :], cs_)
            for e_ in cx.engs:
                e_.wait(tcp)
            nc.all_engine_barrier()
        if "attn" in phases:
            dd = None
            if dbg:
                dd = {}
                for nm, shp, dt in (("d_kT", [1024, S], BF), ("d_qT", [1024, S], BF), ("d_aT", [512, S], BF), ("d_v", [128, S // 128, 16, 65], BF)):
                    dd[nm] = nc.dram_tensor(nm, shp, dt, kind="ExternalOutput").ap()
            emit_attn(cx, g, A, S, y, dbg=dd, stop=stop)
        if "moe1" in phases:
            emit_moe(cx, g, A, S, 1, y)
    return nc, list(A.keys()), cx.rec


def host_inputs(inputs, b, S):
    f = np.float32
    c = inputs["c"][b]
    d = {}
    d["x"] = np.ascontiguousarray(inputs["x"][b, :S])
    d["c_col"] = np.ascontiguousarray(c.reshape(NCH, 128).T)
    d["ident"] = np.eye(128, dtype=f)
    gains = [inputs["norm1_g"][0], inputs["norm1_g"][1], inputs["norm2_g"][0], inputs["norm2_g"][1], inputs["kv_norm_g"]]
    d["ngc"] = np.ascontiguousarray(np.stack([gv.reshape(NCH, 128).T for gv in gains], axis=1))
    d["ada_w"] = inputs["ada_w"]
    d["ada_b"] = inputs["ada_b"]
    d["kv_ada_w"] = inputs["kv_ada_w"]
    d["kv_ada_b"] = inputs["kv_ada_b"].reshape(1, -1)
    d["sgu_w_in"] = inputs["sgu_w_in"][0]
    d["sgu_b_in"] = inputs["sgu_b_in"][0].reshape(1, -1)
    d["sgu_v_g"] = inputs["sgu_v_g"][0]
    d["sgu_w_sT"] = np.ascontiguousarray(inputs["sgu_w_s"][0].transpose(2, 0, 1))
    d["trimask"] = np.triu(np.ones((128, 128), f))
    d["sgu_b_sT"] = np.ascontiguousarray(inputs["sgu_b_s"][0].T)
    d["sgu_w_out"] = inputs["sgu_w_out"][0]
    for l_ in range(2):
        for k in ("moe_w_router", "moe_w_gate", "moe_w_up", "moe_w_down", "moe_b_gate", "moe_b_up", "moe_b_down"):
            d["%s%d" % (k, l_)] = inputs[k][l_]
        d["moe_b_router%d" % l_] = inputs["moe_b_router"][l_].reshape(1, NE)
    import ml_dtypes
    bf = ml_dtypes.bfloat16
    d["w_kv"] = inputs["w_kv"]
    d["attn_w_q"] = inputs["attn_w_q"][0]
    d["attn_w_o"] = inputs["attn_w_o"][0]
    d["kg_t"] = np.tile(inputs["k_norm_g"], 16).astype(f)
    d["qg_t"] = np.tile(inputs["q_norm_g"][0], 16).astype(f)
    slopes = (2.0 ** (-8.0 * np.arange(1, 17, dtype=np.float64) / 16)).astype(f)
    s_hi = slopes.astype(bf).astype(f)
    s_lo = (slopes - s_hi).astype(bf).astype(f)
    qi = np.arange(512)
    Aq = (16 * (qi // 16)).astype(f).reshape(4, 128).T
    Bq = (qi % 16).astype(f).reshape(4, 128).T
    qa = np.zeros((128, 4, 16, 6), f)
    qa[:, :, :, 0] = -Aq[:, :, None]
    qa[:, :, :, 1] = -Bq[:, :, None]
    qa[:, :, :, 2] = -Aq[:, :, None]
    qa[:, :, :, 3] = -Bq[:, :, None]
    qa[:, :, :, 4] = s_hi[None, None, :]
    qa[:, :, :, 5] = s_lo[None, None, :]
    d["qaug_c"] = qa
    pos = np.arange(S)
    ka = np.zeros((16, 32, S), f)
    ka[:, :16, :] = (pos[None, :] // 256 == np.arange(16)[:, None]).astype(f)[None]
    ka[:, 16, :] = s_hi[:, None]
    ka[:, 17, :] = s_hi[:, None]
    ka[:, 18, :] = s_lo[:, None]
    ka[:, 19, :] = s_lo[:, None]
    ka[:, 20, :] = (pos % 128)[None, :]
    ka[:, 21, :] = (pos % 128)[None, :]
    d["kaug_c"] = ka
    ki = np.arange(128)
    cm = np.zeros((128, 4, 512), f)
    for dd in range(4):
        cm[:, dd, :] = np.where(128 * dd + ki[:, None] <= qi[None, :], 0.0, NEG)
    d["cmask"] = cm
    cb = np.zeros((128, 16, 32), f)
    cb[:] = (-slopes[:, None].astype(np.float64) * 128.0 * (np.arange(32)[None, :] - 3)).astype(f)[None]
    d["cbias"] = cb
    d["lstrict"] = np.triu(np.ones((128, 128), f), 1)
    d["tokid"] = (np.arange(32)[None, :] * 128 + np.arange(128)[:, None]).astype(f)
    d["pcol"] = np.arange(128, dtype=f).reshape(128, 1)
    d["jgridb"] = np.ascontiguousarray(np.broadcast_to(np.arange(64, dtype=f)[None, :, None], (128, 64, NE)))
    return d


_CACHE = {}


def kernel(**inputs):
    inputs = {k: np.asarray(v) for k, v in inputs.items()}
    B, S, _ = inputs["x"].shape
    nc, names = build_program(S, phases=("sgu", "moe0", "attn", "moe1"))
    in_maps = []
    for b in range(B):
        d = host_inputs(inputs, b, S)
        in_maps.append({k: d[k] for k in names})
    res = run_bass_kernel_spmd(nc, in_maps, core_ids=list(range(B)))
    return np.stack([r["y"] for r in res.results], axis=0)
```

```python
import numpy as np
from contextlib import ExitStack
import concourse.bass as bass
import concourse.mybir as mybir
from concourse.bass_utils import run_bass_kernel_spmd

F32 = mybir.dt.float32
BF = mybir.dt.bfloat16
I32 = mybir.dt.int32
U32 = mybir.dt.uint32
AF = mybir.ActivationFunctionType
ALU = mybir.AluOpType
AX = mybir.AxisListType

D = 1024
NCH = 8
DS = 3072
NE = 32
EPS = 1e-6
BLK = 512
NEG = -30000.0


class Tok:
    __slots__ = ("sem", "cnt", "key", "eng", "opidx")

    def __init__(self, sem, cnt, key, eng=None, opidx=None):
        self.sem = sem
        self.cnt = cnt
        self.key = key
        self.eng = eng
        self.opidx = opidx


class DSem:
    def __init__(self, cx, name):
        self.sem = cx.take_sem()
        self.cnt = 0
        self.key = name


class Eng:
    def __init__(self, cx, name):
        self.cx = cx
        self.name = name
        self.e = getattr(cx.nc, name)
        self.sem = None
        self.n = 0
        self.seen = {}
        self.key = None
        self.opidx = 0
        self.seen_idx = {}
        self.drained = 0

    def new_phase(self, tag):
        if self.sem is None:
            self.key = "p_%s" % self.name
            self.sem = self.cx.take_sem()
            self.n = 0

    def wait(self, *toks):
        for t in toks:
            if t is None:
                continue
            if isinstance(t, (list, tuple)):
                self.wait(*t)
                continue
            if t.eng is not None:
                if t.eng == self.name:
                    if self.name != "tensor" and t.opidx > self.drained:
                        self.e.drain()
                        self.drained = self.opidx
                    continue
                if self.seen_idx.get(t.eng, 0) >= t.opidx:
                    continue
                self.seen_idx[t.eng] = t.opidx
                if self.cx.needed is None:
                    self.cx.rec.add((t.eng, t.opidx))
                else:
                    assert t.cnt is not None, (t.eng, t.opidx)
                self.e.wait_ge(t.sem, t.cnt)
                continue
            if self.seen.get(t.key, 0) >= t.cnt:
                continue
            self.e.wait_ge(t.sem, t.cnt)
            self.seen[t.key] = t.cnt

    def op(self, fn, *args, deps=(), inc=True, **kw):
        self.wait(*deps)
        ins = getattr(self.e, fn)(*args, **kw)
        if not inc:
            return None
        self.opidx += 1
        if self.cx.needed is None or (self.name, self.opidx) in self.cx.needed:
            self.n += 1
            ins.then_inc(self.sem, 1)
            return Tok(self.sem, self.n, self.key, self.name, self.opidx)
        return Tok(self.sem, None, self.key, self.name, self.opidx)

    def dma(self, out, in_, dsem, deps=(), **kw):
        self.wait(*deps)
        ins = self.e.dma_start(out=out, in_=in_, **kw)
        dsem.cnt += 16
        ins.then_inc(dsem.sem, 16)
        return Tok(dsem.sem, dsem.cnt, dsem.key)


class Ring:
    def __init__(self, bufs):
        self.bufs = bufs
        self.free = [[] for _ in bufs]
        self.i = -1

    def next(self):
        self.i = (self.i + 1) % len(self.bufs)
        return self.bufs[self.i], self.free[self.i], self.i

    def release(self, idx, *toks):
        self.free[idx] = [t for t in toks if t is not None]


class Ctx:
    def __init__(self, nc, needed=None):
        self.nc = nc
        self.needed = needed
        self.rec = set()
        self.es = ExitStack()
        self.pe = Eng(self, "tensor")
        self.dve = Eng(self, "vector")
        self.act = Eng(self, "scalar")
        self.pool = Eng(self, "gpsimd")
        self.sp = Eng(self, "sync")
        self.engs = [self.pe, self.dve, self.act, self.pool, self.sp]
        self.ph = None
        self.nsem = 0
        self.nalloc = 0
        self.pool_free = []
        self.pool_used = []
        self.dsems = []

    def breg(self, val):
        if not hasattr(self, "_bregs"):
            self._bregs = {}
        if val not in self._bregs:
            self._bregs[val] = self.nc.gpsimd.to_reg(val)
        return self._bregs[val]

    def uid(self):
        self.nuid = getattr(self, "nuid", 0) + 1
        return self.nuid

    def take_sem(self):
        if self.pool_free:
            h = self.pool_free.pop()
        else:
            self.nalloc += 1
            h = self.es.enter_context(self.nc.semaphore("sem%d" % self.nalloc))
        self.pool_used.append(h)
        return h

    def dsem(self, name):
        self.nsem += 1
        d = DSem(self, "%s_%d" % (name, self.nsem))
        self.dsems.append(d)
        return d

    def begin_phase(self, tag):
        for e in self.engs:
            e.new_phase(tag)
        self.ph = ExitStack()
        self.ph.__enter__()

    def end_phase(self, final_toks=()):
        for e in self.engs:
            e.wait(*final_toks)
        for d in self.dsems:
            if d.cnt > 0:
                self.sp.wait(Tok(d.sem, d.cnt, d.key))
        self.dsems = []
        self.nc.all_engine_barrier()
        self.ph.__exit__(None, None, None)
        self.ph = None

    def sb(self, name, shape, dt):
        return self.ph.enter_context(self.nc.sbuf_tensor("sb_%s_%d" % (name, self.uid()), list(shape), dt))

    def ps(self, name, shape, dt):
        return self.ph.enter_context(self.nc.psum_tensor("ps_%s_%d" % (name, self.uid()), list(shape), dt))

    def gsb(self, name, shape, dt):
        return self.es.enter_context(self.nc.sbuf_tensor("gsb_" + name, list(shape), dt))


def emit_rstd(cx, ssq, rstd, nfeat, deps):
    t1 = cx.dve.op("tensor_scalar", rstd, ssq, 1.0 / nfeat, EPS, ALU.mult, ALU.add, deps=deps)
    t2 = cx.act.op("activation", rstd, rstd, AF.Sqrt, deps=[t1])
    t3 = cx.dve.op("reciprocal", rstd, rstd, deps=[t2])
    return t3


class G:
    pass


def emit_globals(cx, A, nlayers=2):
    nc = cx.nc
    g = G()
    g.ident_bf = cx.gsb("ident_bf", [128, 128], BF)
    g.ident_f = cx.gsb("ident_f", [128, 128], F32)
    g.ones_bf = cx.gsb("ones_bf", [128, 128], BF)
    g.ones_f = cx.gsb("ones_f", [128, 128], F32)
    g.zeros_bf = cx.gsb("zeros_bf", [128, 128], BF)
    g.modc = cx.gsb("modc", [128, 14, NCH], F32)
    g.gb = cx.gsb("gb", [128, 4, D], F32)
    g.ngc = cx.gsb("ngc", [128, 5, NCH], F32)
    g.gsc = cx.gsb("gsc", [128, 5, NCH], F32)
    g.shc = cx.gsb("shc", [128, 5, NCH], F32)

    cx.begin_phase("g")
    ld = cx.dsem("gld")
    t_id1 = cx.sp.dma(g.ident_f[:], A["ident"], ld)
    t_ng = cx.sp.dma(g.ngc[:], A["ngc"], ld)
    ccol = cx.sb("ccol", [128, NCH], F32)
    t_c = cx.sp.dma(ccol[:], A["c_col"], ld)
    allld = Tok(ld.sem, ld.cnt, ld.key)
    t = cx.dve.op("tensor_copy", g.ident_bf[:], g.ident_f[:], deps=[allld])
    cx.dve.op("memset", g.ones_bf[:], 1.0)
    cx.dve.op("memset", g.zeros_bf[:], 0.0)
    t_ones = cx.dve.op("memset", g.ones_f[:], 1.0)
    cact = cx.sb("cact", [128, NCH], F32)
    t_ca = cx.act.op("activation", cact[:], ccol[:], AF.Silu, deps=[allld])

    NV = 2 * 6 * D + 2 * D
    modrow = cx.sb("modrow", [1, NV], F32)
    brow = cx.sb("brow", [1, NV], F32)
    bl = cx.dsem("gbl")
    cx.sp.dma(brow[0:1, 0:6 * D], A["ada_b"][0:1, :], bl)
    cx.sp.dma(brow[0:1, 6 * D:12 * D], A["ada_b"][1:2, :], bl)
    t_b = cx.sp.dma(brow[0:1, 12 * D:NV], A["kv_ada_b"], bl)
    wring = Ring([cx.sb("adaw%d" % i, [128, NCH, 512], F32) for i in range(2)])
    wsem = [cx.dsem("gw%d" % i) for i in range(2)]
    pr = [cx.ps("g_pr%d" % i, [1, 512], F32) for i in range(2)]
    prfree = [None, None]
    srcs = []
    for l in range(2):
        for n in range(12):
            srcs.append((A["ada_w"][l], n, l * 6 * D + n * 512))
    for n in range(4):
        srcs.append((A["kv_ada_w"], n, 12 * D + n * 512))
    last = None
    for bi, (w, n, off) in enumerate(srcs):
        buf, fr, si = wring.next()
        tl = cx.sp.dma(buf[:], w[:, n * 512:(n + 1) * 512].rearrange("(c p) n -> p c n", p=128), wsem[si], deps=fr)
        p = pr[bi % 2]
        tm = None
        for k in range(NCH):
            tm = cx.pe.op("matmul", p[0:1, :], cact[:, k:k + 1], buf[:, k, :], start=(k == 0), stop=(k == NCH - 1),
                          deps=[tl, t_ca, prfree[bi % 2]] if k == 0 else (), inc=(k == NCH - 1))
        wring.release(si, tm)
        te = cx.dve.op("tensor_tensor", modrow[0:1, off:off + 512], p[0:1, :], brow[0:1, off:off + 512], ALU.add,
                       deps=[tm, t_b])
        prfree[bi % 2] = te
        last = te
    pc = cx.ps("g_pc", [128, 14 * NCH], F32)
    vec_off = []
    for l in range(2):
        for j in range(6):
            vec_off.append(l * 6 * D + j * D)
    vec_off += [12 * D, 13 * D]
    tm = None
    for vi, off in enumerate(vec_off):
        for c in range(NCH):
            tm = cx.pe.op("matmul", pc[:, vi * NCH + c: vi * NCH + c + 1], modrow[0:1, off + c * 128: off + (c + 1) * 128],
                          g.ones_f[0:1, 0:1], start=True, stop=True, deps=[last, t_ones] if (vi == 0 and c == 0) else (),
                          inc=(vi == 13 and c == NCH - 1))
    t_mc = cx.dve.op("tensor_copy", g.modc[:].rearrange("p v c -> p (v c)"), pc[:], deps=[tm])
    pb = [cx.ps("g_pb%d" % i, [128, 512], F32) for i in range(2)]
    pbfree = [None, None]
    k = 0
    tl2 = t_mc
    for gi, off in enumerate([2 * D, 5 * D, 6 * D + 2 * D, 6 * D + 5 * D]):
        for hf in range(2):
            tm = cx.pe.op("matmul", pb[k % 2][:], g.ones_f[0:1, :], modrow[0:1, off + hf * 512: off + (hf + 1) * 512],
                          start=True, stop=True, deps=[pbfree[k % 2]])
            pbfree[k % 2] = cx.dve.op("tensor_copy", g.gb[:, gi, hf * 512:(hf + 1) * 512], pb[k % 2][:], deps=[tm])
            tl2 = pbfree[k % 2]
            k += 1
    pairs = [(0, 0, 1), (2, 3, 4), (1, 6, 7), (3, 9, 10), (4, 12, 13)]
    tt = None
    for oi, (gi, shv, scv) in enumerate(pairs):
        t1 = cx.dve.op("scalar_tensor_tensor", g.gsc[:, oi, :], g.modc[:, scv, :], 1.0, g.ngc[:, gi, :], ALU.add, ALU.mult,
                       deps=[t_mc, allld])
        tt = cx.dve.op("tensor_copy", g.shc[:, oi, :], g.modc[:, shv, :], deps=[t_mc])
    cx.end_phase([tt, tl2, t])
    return g


def emit_norm_T(cx, g, xt, oi, hT, pT, work, deps, pT_free):
    junk, ssq, rstd, xn = work["junk"], work["ssq"], work["rstd"], work["xn"]
    t = cx.act.op("activation", junk[:, 0:D], xt, AF.Square, accum_out=ssq[:], deps=deps)
    t = emit_rstd(cx, ssq[:], rstd[:], D, [t])
    t_xn = cx.act.op("activation", xn[:], xt, AF.Copy, scale=rstd[:], deps=[t])
    tm = None
    for c in range(NCH):
        tm = cx.pe.op("transpose", pT[:, c, :], xn[:, c * 128:(c + 1) * 128], g.ident_bf[:],
                      deps=[t_xn, pT_free] if c == 0 else (), inc=(c == NCH - 1))
    te = None
    for c in range(NCH):
        te = cx.dve.op("tensor_scalar", hT[:, c, :], pT[:, c, :], g.gsc[:, oi, c:c + 1], g.shc[:, oi, c:c + 1],
                       ALU.mult, ALU.add, deps=[tm] if c == 0 else ())
    return te, t_xn


def emit_sgu(cx, g, A, S, x_in, x_out):
    nc = cx.nc
    NT = S // 128
    yT_scr = nc.dram_tensor("yT_scr", [128, 24, S], BF, kind="Internal").ap()
    cx.begin_phase("s1")
    w_in = cx.sb("w_in", [128, NCH, 2 * DS], BF)
    b_in = cx.sb("b_in", [1, 2 * DS], BF)
    vgb = cx.sb("vgb", [128, DS], F32)
    wsT = cx.sb("wsT", [128, 8, 128], BF)
    wsTf = cx.sb("wsTf", [128, 8, 128], F32)
    trimask = cx.sb("trimask", [128, 128], F32)
    bsT = cx.sb("bsT", [128, 8], F32)
    wl = cx.dsem("s1w")
    for c in range(NCH):
        cx.pool.dma(w_in[:, c, :], A["sgu_w_in"][c * 128:(c + 1) * 128, :], wl, max_dma_last_dim=4096)
    cx.pool.dma(b_in[:], A["sgu_b_in"], wl, max_dma_last_dim=4096)
    cx.sp.dma(vgb[:], A["sgu_v_g"].partition_broadcast(128), wl)
    cx.sp.dma(wsTf[:], A["sgu_w_sT"], wl)
    cx.sp.dma(trimask[:], A["trimask"], wl)
    cx.sp.dma(bsT[:], A["sgu_b_sT"], wl)
    t_w = Tok(wl.sem, wl.cnt, wl.key)
    t_ws = None
    for gi in range(8):
        t_ws = cx.dve.op("tensor_tensor", wsT[:, gi, :], wsTf[:, gi, :], trimask[:], ALU.mult, deps=[t_w])

    xr = Ring([cx.sb("s1x%d" % i, [128, D], F32) for i in range(2)])
    xsem = [cx.dsem("s1x%d" % i) for i in range(2)]
    work = dict(junk=cx.sb("s1junk", [128, DS], BF), ssq=cx.sb("s1ssq", [128, 1], F32),
                rstd=cx.sb("s1rstd", [128, 1], F32), xn=cx.sb("s1xn", [128, D], BF))
    hT = cx.sb("s1hT", [128, NCH, 128], BF)
    u = cx.sb("s1u", [128, DS], BF)
    v = cx.sb("s1v", [128, DS], BF)
    vn = cx.sb("s1vn", [128, DS], BF)
    y = cx.sb("s1y", [128, DS], BF)
    yT = Ring([cx.sb("s1yT%d" % i, [128, 24, 128], BF) for i in range(1)])
    ysem = [cx.dsem("s1y%d" % i) for i in range(1)]
    ssqv = cx.sb("s1ssqv", [128, 1], F32)
    rstdv = cx.sb("s1rstdv", [128, 1], F32)
    pT = cx.ps("s1pT", [128, NCH, 128], BF)
    pz = [cx.ps("s1pz%d" % i, [128, 512], F32) for i in range(2)]
    pm = [cx.ps("s1pm%d" % i, [128, 512], F32) for i in range(2)]
    pyT = cx.ps("s1pyT", [128, 24, 128], BF)
    pz_free = [None, None]
    pm_free = [None, None]
    pT_free = None
    pyT_free = None
    hT_free = None
    loads = {}

    def load(i):
        buf, fr, si = xr.next()
        loads[i] = (buf, si, cx.sp.dma(buf[:], x_in[i * 128:(i + 1) * 128, :], xsem[si], deps=fr))

    load(0)
    store_toks = []
    for i in range(NT):
        if i + 1 < NT:
            load(i + 1)
        xt, xsi, tl = loads.pop(i)
        te, t_xn = emit_norm_T(cx, g, xt[:], 0, hT, pT, work, [tl, hT_free], pT_free)
        pT_free = te
        xr.release(xsi, t_xn)
        tz_last = None
        t_gl = []
        for n in range(12):
            p = pz[n % 2]
            for k in range(NCH):
                cx.pe.op("matmul", p[:], hT[:, k, :], w_in[:, k, n * 512:(n + 1) * 512], start=(k == 0), stop=False,
                         deps=[te, t_w, pz_free[n % 2]] if k == 0 else (), inc=False)
            tm = cx.pe.op("matmul", p[:], g.ones_bf[0:1, :], b_in[0:1, n * 512:(n + 1) * 512], start=False, stop=True)
            if n < 6:
                tg = cx.act.op("activation", u[:, n * 512:(n + 1) * 512], p[:], AF.Gelu_apprx_tanh, deps=[tm])
            else:
                tg = cx.act.op("activation", v[:, (n - 6) * 512:(n - 5) * 512], p[:], AF.Gelu_apprx_tanh, deps=[tm])
            pz_free[n % 2] = tg
            t_gl.append(tg)
            tz_last = tm
        hT_free = tz_last
        t = cx.act.op("activation", work["junk"][:], v[:], AF.Square, accum_out=ssqv[:], deps=[t_gl[-1]])
        t = emit_rstd(cx, ssqv[:], rstdv[:], DS, [t])
        t_vn = cx.dve.op("scalar_tensor_tensor", vn[:], v[:], rstdv[:], vgb[:], ALU.mult, ALU.mult, deps=[t, t_w])
        t_y = None
        for gi in range(8):
            p = pm[gi % 2]
            tm = cx.pe.op("matmul", p[:, 0:384], wsT[:, gi, :], vn[:, gi * 384:(gi + 1) * 384], start=True, stop=True,
                          deps=[t_vn, t_ws, pm_free[gi % 2]])
            t_y = cx.dve.op("scalar_tensor_tensor", y[:, gi * 384:(gi + 1) * 384], p[:, 0:384], bsT[:, gi:gi + 1],
                            u[:, gi * 384:(gi + 1) * 384], ALU.add, ALU.mult, deps=[tm, t_gl[5]])
            pm_free[gi % 2] = t_y
        tm = None
        for c in range(24):
            tm = cx.pe.op("transpose", pyT[:, c, :], y[:, c * 128:(c + 1) * 128], g.ident_bf[:],
                          deps=[t_y, pyT_free] if c == 0 else (), inc=(c == 23))
        ybuf, yfr, ysi = yT.next()
        te2 = None
        for q in range(3):
            te2 = cx.act.op("activation", ybuf[:, q * 8:(q + 1) * 8, :], pyT[:, q * 8:(q + 1) * 8, :], AF.Copy,
                            deps=[tm] + yfr if q == 0 else ())
        pyT_free = te2
        ts = cx.sp.dma(yT_scr[:, :, i * 128:(i + 1) * 128], ybuf[:], ysem[ysi], deps=[te2])
        yT.release(ysi, ts)
        store_toks.append(ts)
    cx.end_phase(store_toks[-2:])

    cx.begin_phase("s2")
    w_out = cx.sb("w_out", [128, 24, D], BF)
    wl = cx.dsem("s2w")
    for c in range(24):
        cx.pool.dma(w_out[:, c, :], A["sgu_w_out"][c * 128:(c + 1) * 128, :], wl, max_dma_last_dim=4096)
    t_w = Tok(wl.sem, wl.cnt, wl.key)
    xr = Ring([cx.sb("s2x%d" % i, [128, D], F32) for i in range(2)])
    yr = Ring([cx.sb("s2y%d" % i, [128, 24, 128], BF) for i in range(2)])
    lsem = [cx.dsem("s2l%d" % i) for i in range(2)]
    orr = Ring([cx.sb("s2o%d" % i, [128, D], F32) for i in range(2)])
    osem = [cx.dsem("s2o%d" % i) for i in range(2)]
    po = [cx.ps("s2po%d" % i, [128, 512], F32) for i in range(4)]
    po_free = [None] * 4
    loads = {}

    def load2(i):
        xb, xf, si = xr.next()
        yb, yf, _ = yr.next()
        cx.sp.dma(xb[:], x_in[i * 128:(i + 1) * 128, :], lsem[si], deps=xf + yf)
        tl = cx.sp.dma(yb[:], yT_scr[:, :, i * 128:(i + 1) * 128], lsem[si])
        loads[i] = (xb, yb, si, tl)

    load2(0)
    store_toks = []
    for i in range(NT):
        if i + 1 < NT:
            load2(i + 1)
        xb, yb, si, tl = loads.pop(i)
        ob, ofr, osi = orr.next()
        tms = []
        for hf in range(2):
            p = po[(i % 2) * 2 + hf]
            tm = None
            for k in range(24):
                tm = cx.pe.op("matmul", p[:], yb[:, k, :], w_out[:, k, hf * 512:(hf + 1) * 512], start=(k == 0),
                              stop=(k == 23), deps=[tl, t_w, po_free[(i % 2) * 2 + hf]] if k == 0 else (), inc=(k == 23))
            t1 = cx.dve.op("tensor_tensor", ob[:, hf * 512:(hf + 1) * 512], p[:], g.gb[:, 0, hf * 512:(hf + 1) * 512],
                           ALU.mult, deps=[tm] + ofr)
            po_free[(i % 2) * 2 + hf] = t1
            tms.append(tm)
        t2 = cx.dve.op("tensor_tensor", ob[:], ob[:], xb[:], ALU.add, deps=[t1])
        yr.release(si, tms[-1])
        xr.release(si, t2)
        ts = cx.sp.dma(x_out[i * 128:(i + 1) * 128, :], ob[:], osem[osi], deps=[t2])
        orr.release(osi, ts)
        store_toks.append(ts)
    cx.end_phase(store_toks[-2:])


def emit_moe(cx, g, A, S, l, xio, dbg=None, stop=None):
    nc = cx.nc
    NT = S // 128
    NBLK = (S * 4) // BLK + NE
    R = NBLK * BLK
    oi = 1 if l == 0 else 3
    h2_scr = nc.dram_tensor("h2_scr%d" % l, [S, D], BF, kind="Internal").ap()
    rowinfo = nc.dram_tensor("rowinfo%d" % l, [R, 2], F32, kind="Internal").ap()
    yrows = nc.dram_tensor("yrows%d" % l, [R, D], F32, kind="Internal").ap()
    mes = ExitStack()
    mes.__enter__()
    Pd = mes.enter_context(nc.sbuf_tensor("sb_Pd%d" % l, [128, NT, NE], F32))
    didx = mes.enter_context(nc.sbuf_tensor("sb_didx%d" % l, [128, NT, 4], I32))
    idxw = mes.enter_context(nc.sbuf_tensor("sb_idxw%d" % l, [128, NBLK], I32))
    idxw4 = mes.enter_context(nc.sbuf_tensor("sb_idxw4%d" % l, [128, NBLK, 4], I32))

    cx.begin_phase("r%d" % l)
    wr = cx.sb("wr", [128, NCH, NE], F32)
    brb = cx.sb("brb", [128, NE], F32)
    lstrict = cx.sb("lstrict", [128, 128], BF)
    lsf = cx.sb("lsf", [128, 128], F32)
    tokid = cx.sb("tokid", [128, NT], F32)
    pcol = cx.sb("pcol", [128, 1], F32)
    jgrid = cx.sb("jgrid", [128, NBLK, NE], F32)
    gsb2 = cx.sb("gsb2", [128, D], F32)
    shb2 = cx.sb("shb2", [128, D], F32)
    cl = cx.dsem("rc")
    cx.sp.dma(wr[:], A["moe_w_router%d" % l].rearrange("(c p) e -> p c e", p=128), cl)
    cx.sp.dma(brb[:], A["moe_b_router%d" % l].rearrange("o e -> (o e)").partition_broadcast(128), cl)
    cx.sp.dma(lsf[:], A["lstrict"], cl)
    cx.sp.dma(tokid[:], A["tokid"][:, 0:NT], cl)
    cx.sp.dma(pcol[:], A["pcol"], cl)
    cx.sp.dma(jgrid[:], A["jgridb"][:, 0:NBLK, :], cl)
    t_c = Tok(cl.sem, cl.cnt, cl.key)
    t_ls = cx.dve.op("tensor_copy", lstrict[:], lsf[:], deps=[t_c])
    diag = cx.sb("diag", [128, 128], F32)
    pbt = cx.ps("r_pb", [128, 512], F32)
    t_free = None
    t_bc = None
    for which, dst in ((g.gsc, gsb2), (g.shc, shb2)):
        for q in range(2):
            tm = None
            tds = []
            for cc in range(4):
                c = q * 4 + cc
                td = cx.dve.op("tensor_scalar", diag[:], g.ident_f[:], which[:, oi, c:c + 1], None, ALU.mult,
                               deps=[tm, t_c])
                tm = cx.pe.op("matmul", pbt[:, cc * 128:(cc + 1) * 128], g.ones_f[:], diag[:], start=True, stop=True,
                              deps=[td, t_free] if cc == 0 else [td])
            t_free = cx.dve.op("tensor_copy", dst[:, q * 512:(q + 1) * 512], pbt[:], deps=[tm])
            t_bc = t_free

    xr = Ring([cx.sb("rx%d" % i, [128, D], F32) for i in range(2)])
    xsem = [cx.dsem("rx%d" % i) for i in range(2)]
    junk = cx.sb("rjunk", [128, D], BF)
    ssq = cx.sb("rssq", [128, 1], F32)
    rstd = cx.sb("rrstd", [128, 1], F32)
    xn = cx.sb("rxn", [128, D], F32)
    tmp = cx.sb("rtmp", [128, D], F32)
    h2r = Ring([cx.sb("rh2%d" % i, [128, D], BF) for i in range(2)])
    hsem = [cx.dsem("rh%d" % i) for i in range(2)]
    h2Th = cx.sb("rh2Th", [128, NCH, 128], BF)
    h2Tl = cx.sb("rh2Tl", [128, NCH, 128], BF)
    hlo = cx.sb("rhlo", [128, D], BF)
    wrh = cx.sb("rwrh", [128, NCH, NE], BF)
    wrl = cx.sb("rwrl", [128, NCH, NE], BF)
    t_wr = cx.dve.op("tensor_copy", wrh[:], wr[:], deps=[t_c])
    t_wr = cx.dve.op("tensor_tensor", wrl[:], wr[:], wrh[:], ALU.subtract, deps=[t_wr])
    lg = cx.sb("rlg", [128, NE], F32)
    m8 = cx.sb("rm8", [128, 8], F32)
    negmax = cx.sb("rnm", [128, 1], F32)
    ex = cx.sb("rex", [128, NE], F32)
    ssum = cx.sb("rssum", [128, 1], F32)
    maskf = cx.sb("rmaskf", [128, NT, NE], F32)
    maskb = cx.sb("rmaskb", [128, NT, NE], BF)
    rank = cx.sb("rrank", [128, NT, NE], F32)
    pTh = cx.ps("r_pTh", [128, NCH, 128], BF)
    pTl = cx.ps("r_pTl", [128, NCH, 128], BF)
    plg = cx.ps("r_plg", [128, NE], F32)
    prk = cx.ps("r_prk", [128, NE], F32)
    pTf_free = None
    plg_free = None
    prk_free = None
    h2T_free = None
    loads = {}

    def load(i):
        buf, fr, si = xr.next()
        loads[i] = (buf, si, cx.sp.dma(buf[:], xio[i * 128:(i + 1) * 128, :], xsem[si], deps=fr))

    load(0)
    st = []
    for i in range(NT):
        if i + 1 < NT:
            load(i + 1)
        xt, xsi, tl = loads.pop(i)
        t = cx.act.op("activation", junk[:], xt[:], AF.Square, accum_out=ssq[:], deps=[tl])
        t = emit_rstd(cx, ssq[:], rstd[:], D, [t])
        t_xn = cx.act.op("activation", xn[:], xt[:], AF.Copy, scale=rstd[:], deps=[t, h2T_free])
        xr.release(xsi, t_xn)
        hb, hfr, hsi = h2r.next()
        t1 = cx.dve.op("tensor_tensor", tmp[:], xn[:], gsb2[:], ALU.mult, deps=[t_xn, t_bc])
        t1 = cx.dve.op("tensor_tensor", tmp[:], tmp[:], shb2[:], ALU.add, deps=[t1])
        t2 = cx.dve.op("tensor_copy", hb[:], tmp[:], deps=[t1] + hfr)
        t3 = cx.dve.op("tensor_tensor", hlo[:], tmp[:], hb[:], ALU.subtract, deps=[t2, h2T_free])
        ts = cx.sp.dma(h2_scr[i * 128:(i + 1) * 128, :], hb[:], hsem[hsi], deps=[t2])
        st.append(ts)
        tm = None
        for c in range(NCH):
            tm = cx.pe.op("transpose", pTh[:, c, :], hb[:, c * 128:(c + 1) * 128], g.ident_bf[:],
                          deps=[t2, pTf_free] if c == 0 else (), inc=(c == NCH - 1))
        tm2 = None
        for c in range(NCH):
            tm2 = cx.pe.op("transpose", pTl[:, c, :], hlo[:, c * 128:(c + 1) * 128], g.ident_bf[:],
                           deps=[t3] if c == 0 else (), inc=(c == NCH - 1))
        h2r.release(hsi, ts, tm)
        te1 = cx.act.op("activation", h2Th[:], pTh[:], AF.Copy, deps=[tm, h2T_free])
        te = cx.act.op("activation", h2Tl[:], pTl[:], AF.Copy, deps=[tm2])
        pTf_free = te
        k = 0
        for c in range(NCH):
            for (lh, rh) in ((h2Th, wrh), (h2Th, wrl), (h2Tl, wrh)):
                tm = cx.pe.op("matmul", plg[:], lh[:, c, :], rh[:, c, :], start=(k == 0), stop=(k == 3 * NCH - 1),
                              deps=[te, t_wr, plg_free] if k == 0 else (), inc=(k == 3 * NCH - 1))
                k += 1
        h2T_free = tm
        t = cx.dve.op("tensor_tensor", lg[:], plg[:], brb[:], ALU.add, deps=[tm, t_c])
        plg_free = t
        t = cx.dve.op("max", m8[:], lg[:], deps=[t])
        t_m = cx.dve.op("tensor_scalar", maskf[:, i, :], lg[:], m8[:, 3:4], None, ALU.is_ge, deps=[t])
        t_mb = cx.dve.op("tensor_copy", maskb[:, i, :], maskf[:, i, :], deps=[t_m])
        t_n = cx.dve.op("tensor_scalar", negmax[:], m8[:, 0:1], -1.0, None, ALU.mult, deps=[t])
        t_e = cx.act.op("activation", ex[:], lg[:], AF.Exp, bias=negmax[:], deps=[t_n])
        t = cx.dve.op("tensor_tensor", ex[:], ex[:], maskf[:, i, :], ALU.mult, deps=[t_e, t_m])
        t = cx.dve.op("tensor_reduce", ssum[:], ex[:], AX.X, ALU.add, deps=[t])
        t = cx.dve.op("reciprocal", ssum[:], ssum[:], deps=[t])
        t = cx.dve.op("tensor_scalar", Pd[:, i, :], ex[:], ssum[:, 0:1], None, ALU.mult, deps=[t])
        mms = [(lstrict, i)] + [(g.ones_bf, ii) for ii in range(i)]
        tmr = None
        for qi, (lhs, ii) in enumerate(mms):
            lastq = qi == len(mms) - 1
            tmr = cx.pe.op("matmul", prk[:], lhs[:], maskb[:, ii, :], start=(qi == 0), stop=lastq,
                           deps=[t_mb, t_ls, prk_free] if qi == 0 else (), inc=lastq)
        prk_free = cx.dve.op("tensor_copy", rank[:, i, :], prk[:], deps=[tmr])
    for ii in range(NT):
        tm = cx.pe.op("matmul", prk[:], g.ones_bf[:], maskb[:, ii, :], start=(ii == 0), stop=(ii == NT - 1),
                      deps=[prk_free] if ii == 0 else (), inc=(ii == NT - 1))
    cnt = cx.sb("rcnt", [128, NE], F32)
    nb = cx.sb("rnb", [128, NE], F32)
    pend = cx.sb("rpend", [128, NE], F32)
    prow = cx.sb("rprow", [128, NE], F32)
    onesr = cx.sb("rones", [128, NE], F32)
    t = cx.dve.op("tensor_copy", cnt[:], prk[:], deps=[tm])
    t = cx.dve.op("memset", onesr[:], 1.0, deps=[t])
    t = cx.dve.op("tensor_scalar", nb[:], cnt[:], 0.0, None, ALU.is_gt, deps=[t])
    for m in range(1, S // BLK):
        t = cx.dve.op("scalar_tensor_tensor", nb[:], cnt[:], float(m * BLK), nb[:], ALU.is_gt, ALU.add, deps=[t])
    t = cx.dve.op("tensor_tensor_scan", pend[:], onesr[:], nb[:], 0.0, ALU.mult, ALU.add, deps=[t])
    t = cx.dve.op("tensor_tensor", prow[:], pend[:], nb[:], ALU.subtract, deps=[t])
    t = cx.dve.op("tensor_scalar", prow[:], prow[:], float(BLK), None, ALU.mult, deps=[t])
    t = cx.dve.op("tensor_tensor", rank[:], rank[:], prow[:].unsqueeze(1).to_broadcast([128, NT, NE]), ALU.add, deps=[t])
    t = cx.dve.op("scalar_tensor_tensor", rank[:], rank[:], 1.0, maskf[:], ALU.add, ALU.mult, deps=[t])
    t = cx.dve.op("tensor_scalar", rank[:], rank[:], -1.0, None, ALU.add, deps=[t])
    top8 = cx.sb("rtop8", [128, NT, 8], F32)
    for i in range(NT):
        t = cx.dve.op("max", top8[:, i, :], rank[:, i, :], deps=[t] if i == 0 else ())
    t_top = cx.dve.op("tensor_copy", didx[:], top8[:, :, 0:4], deps=[t])
    sc = cx.sb("rsc", [128, NT, 4, 2], F32)
    eq = cx.sb("req", [128, NT, NE], F32)
    tsc = None
    for k in range(4):
        t = cx.dve.op("tensor_tensor", eq[:], rank[:], top8[:, :, k:k + 1].to_broadcast([128, NT, NE]), ALU.is_equal,
                      deps=[t_top])
        t = cx.dve.op("tensor_tensor", eq[:], eq[:], Pd[:], ALU.mult, deps=[t])
        t = cx.dve.op("tensor_reduce", sc[:, :, k, 1], eq[:], AX.X, ALU.add, deps=[t])
        tsc = cx.dve.op("tensor_copy", sc[:, :, k, 0], tokid[:], deps=[t])
    cmp3 = cx.sb("rcmp", [128, NBLK, NE], F32)
    be = cx.sb("rbe", [128, NBLK], F32)
    t = cx.dve.op("tensor_tensor", cmp3[:], pend[:].unsqueeze(1).to_broadcast([128, NBLK, NE]), jgrid[:], ALU.is_le,
                  deps=[tsc])
    t = cx.dve.op("tensor_reduce", be[:], cmp3[:], AX.X, ALU.add, deps=[t])
    unu = cx.sb("runu", [128, NBLK], F32)
    t = cx.dve.op("tensor_scalar", unu[:], be[:], float(NE), 1.0e6, ALU.is_ge, ALU.mult, deps=[t])
    t = cx.dve.op("tensor_scalar", be[:], be[:], float(NE - 1), 128.0, ALU.min, ALU.mult, deps=[t])
    t = cx.dve.op("tensor_scalar", be[:], be[:], pcol[:, 0:1], None, ALU.add, deps=[t])
    be4 = cx.sb("rbe4", [128, NBLK], F32)
    t = cx.dve.op("tensor_tensor", be4[:], be[:], unu[:], ALU.add, deps=[t])
    t_iw = cx.dve.op("tensor_copy", idxw[:], be4[:], deps=[t])
    for h in range(4):
        t = cx.dve.op("tensor_scalar", be4[:], be[:], 4.0, float(h), ALU.mult, ALU.add, deps=[t_iw])
        t = cx.dve.op("tensor_tensor", be4[:], be4[:], unu[:], ALU.add, deps=[t])
        t_iw = cx.dve.op("tensor_copy", idxw4[:, :, h], be4[:], deps=[t])
    zt = cx.sb("rzt", [128, NBLK * 8], F32)
    tz = cx.dve.op("memset", zt[:], 0.0)
    zs = cx.dsem("rz")
    t_z = cx.sp.dma(rowinfo.rearrange("(p a) c -> p (a c)", p=128), zt[:], zs, deps=[tz])
    ss = cx.dsem("rs")
    cx.pool.wait(t_z, tsc, t_top)
    for i in range(NT):
        for k in range(4):
            ins = cx.pool.e.indirect_dma_start(out=rowinfo, out_offset=bass.IndirectOffsetOnAxis(ap=didx[:, i, k:k + 1], axis=0),
                                               in_=sc[:, i, k, :], in_offset=None, bounds_check=cx.breg(R - 1), oob_is_err=False)
            ss.cnt += 16
            ins.then_inc(ss.sem, 16)
    t_sc = Tok(ss.sem, ss.cnt, ss.key)
    dtoks = []
    if dbg is not None:
        ds_ = cx.dsem("dbg")
        cx.sp.wait(t_sc, t_iw)
        dtoks.append(cx.sp.dma(dbg["d_didx"], didx[:], ds_))
        dtoks.append(cx.sp.dma(dbg["d_idxw"], idxw[:], ds_))
        dtoks.append(cx.sp.dma(dbg["d_Pd"], Pd[:], ds_))
        dtoks.append(cx.sp.dma(dbg["d_rowinfo"], rowinfo, ds_))
        dtoks.append(cx.sp.dma(dbg["d_cnt"], cnt[:], ds_))
        dtoks.append(cx.sp.dma(dbg["d_h2"], h2_scr, ds_))
        dtoks = [Tok(ds_.sem, ds_.cnt, ds_.key)]
    cx.end_phase([t_sc, t_iw] + st[-2:] + dtoks)
    if stop == "R":
        mes.__exit__(None, None, None)
        return

    cx.begin_phase("e%d" % l)
    Wg = A["moe_w_gate%d" % l].rearrange("e (p h c) n -> (e p h) (c n)", h=4, c=2)
    Wu = A["moe_w_up%d" % l].rearrange("e (p h c) n -> (e p h) (c n)", h=4, c=2)
    Wd = A["moe_w_down%d" % l].rearrange("e (p h c) n -> (e p h) (c n)", h=4, c=2)
    Bg = A["moe_b_gate%d" % l].rearrange("e (p c) -> (e p) c", c=8)
    Bu = A["moe_b_up%d" % l].rearrange("e (p c) -> (e p) c", c=8)
    wgs = [cx.sb("wg%d" % i, [128, NCH, D], BF) for i in range(2)]
    wus = [cx.sb("wu%d" % i, [128, NCH, D], BF) for i in range(2)]
    wds = [cx.sb("wd%d" % i, [128, NCH, D], BF) for i in range(2)]
    bgs = [cx.sb("bg%d" % i, [128, NCH], F32) for i in range(2)]
    bus = [cx.sb("bu%d" % i, [128, NCH], F32) for i in range(2)]
    wsem = [cx.dsem("ew%d" % i) for i in range(2)]
    w_free = [[], []]
    ris = [cx.sb("ri%d" % i, [128, 4, 2], F32) for i in range(2)]
    tixs = [cx.sb("tix%d" % i, [128, 4], I32) for i in range(2)]
    risem = [cx.dsem("eri%d" % i) for i in range(2)]
    ri_free = [[], []]
    xgs = [cx.sb("xg%d" % i, [128, 4, D], BF) for i in range(2)]
    xgsem = [cx.dsem("exg%d" % i) for i in range(2)]
    xg_free = [[], []]
    xT = cx.sb("exT", [128, NCH, BLK], BF)
    a = cx.sb("ea", [128, NCH, BLK], BF)
    gc = [cx.sb("egc%d" % i, [128, BLK], F32) for i in range(2)]
    sg = [cx.sb("esg%d" % i, [128, BLK], F32) for i in range(2)]
    u1 = [cx.sb("eu1%d" % i, [128, BLK], F32) for i in range(2)]
    ysb = [cx.sb("ey%d" % i, [128, D], F32) for i in range(2)]
    ysem = [cx.dsem("ey%d" % i) for i in range(2)]
    y_free = [[], []]
    ptr = [cx.ps("e_ptr%d" % i, [128, 2 * BLK], BF) for i in range(2)]
    pg = [cx.ps("e_pg%d" % i, [128, BLK], F32) for i in range(2)]
    pu = [cx.ps("e_pu%d" % i, [128, BLK], F32) for i in range(2)]
    pd = [cx.ps("e_pd%d" % i, [128, BLK], F32) for i in range(2)]
    ptr_free = [None, None]
    pg_free = [None, None]
    pu_free = [None, None]
    pd_free = [None, None]
    xT_free = None
    a_free = None
    tmp_free = [None, None]
    pre = {}

    def prefetch(j):
        s_ = j % 2
        cx.pool.wait(w_free[s_])
        toks = []
        for dst, src in ((wgs[s_], Wg), (wus[s_], Wu), (wds[s_], Wd)):
            for h in range(4):
                ins = cx.pool.e.indirect_dma_start(out=dst[:, 2 * h:2 * h + 2, :].rearrange("p c n -> p (c n)"),
                                                   out_offset=None, in_=src,
                                                   in_offset=bass.IndirectOffsetOnAxis(ap=idxw4[:, j, h:h + 1], axis=0),
                                                   bounds_check=cx.breg(NE * 128 * 4 - 1), oob_is_err=False)
                wsem[s_].cnt += 16
                ins.then_inc(wsem[s_].sem, 16)
        for dst, src in ((bgs[s_], Bg), (bus[s_], Bu)):
            ins = cx.pool.e.indirect_dma_start(out=dst[:], out_offset=None, in_=src,
                                               in_offset=bass.IndirectOffsetOnAxis(ap=idxw[:, j:j + 1], axis=0),
                                               bounds_check=cx.breg(NE * 128 - 1), oob_is_err=False)
            wsem[s_].cnt += 16
            ins.then_inc(wsem[s_].sem, 16)
        t_w = Tok(wsem[s_].sem, wsem[s_].cnt, wsem[s_].key)
        t_ri = cx.sp.dma(ris[s_][:], rowinfo[j * BLK:(j + 1) * BLK, :].rearrange("(r p) c -> p r c", p=128), risem[s_],
                         deps=ri_free[s_])
        t_ix = cx.dve.op("tensor_copy", tixs[s_][:], ris[s_][:, :, 0], deps=[t_ri])
        cx.pool.wait(t_ix, xg_free[s_])
        for r in range(4):
            ins = cx.pool.e.indirect_dma_start(out=xgs[s_][:, r, :], out_offset=None, in_=h2_scr,
                                               in_offset=bass.IndirectOffsetOnAxis(ap=tixs[s_][:, r:r + 1], axis=0),
                                               bounds_check=cx.breg(S - 1), oob_is_err=False)
            xgsem[s_].cnt += 16
            ins.then_inc(xgsem[s_].sem, 16)
        t_xg = Tok(xgsem[s_].sem, xgsem[s_].cnt, xgsem[s_].key)
        pre[j] = (t_w, t_ri, t_xg)

    prefetch(0)
    yst = []
    nev = 0
    for j in range(NBLK):
        if j + 1 < NBLK:
            prefetch(j + 1)
        s_ = j % 2
        t_w, t_ri, t_xg = pre.pop(j)
        wg_, wu_, wd_, bg_, bu_, xg_, ri_ = wgs[s_], wus[s_], wds[s_], bgs[s_], bus[s_], xgs[s_], ris[s_]
        tev = None
        for c in range(NCH):
            p = ptr[c % 2]
            tm = None
            for r in range(4):
                tm = cx.pe.op("transpose", p[:, r * 128:(r + 1) * 128],
                              xg_[:, r, :].rearrange("t (p c) -> t c p", c=8)[:, c, :], g.ident_bf[:],
                              deps=[t_xg, ptr_free[c % 2], xT_free] if r == 0 else (), inc=(r == 3))
            if c % 2 == 0:
                tev = cx.act.op("activation", xT[:, c, :], p[:, 0:BLK], AF.Copy, deps=[tm])
            else:
                tev = cx.dve.op("tensor_copy", xT[:, c, :], p[:, 0:BLK], deps=[tm])
            ptr_free[c % 2] = tev
            if c == NCH - 2:
                tev_a = tev
        xg_free[s_] = [tm]
        t_xT = [tev, tev_a]
        t_a = None
        tm_last = None
        for m in range(NCH):
            q = m % 2
            for k in range(NCH):
                cx.pe.op("matmul", pg[q][:], wg_[:, k, :].rearrange("q (p c) -> q c p", c=8)[:, m, :], xT[:, k, :],
                         start=(k == 0), stop=(k == NCH - 1),
                         deps=[t_w, t_xT, pg_free[q], a_free] if k == 0 else (), inc=False)
            for k in range(NCH):
                tm = cx.pe.op("matmul", pu[q][:], wu_[:, k, :].rearrange("q (p c) -> q c p", c=8)[:, m, :], xT[:, k, :],
                              start=(k == 0), stop=(k == NCH - 1), deps=[pu_free[q]] if k == 0 else (),
                              inc=(k == NCH - 1))
            tm_last = tm
            t1 = cx.dve.op("tensor_scalar", gc[q][:], pg[q][:], bg_[:, m:m + 1], 7.0, ALU.add, ALU.min,
                           deps=[tm, tmp_free[q]])
            pg_free[q] = t1
            t2 = cx.act.op("activation", sg[q][:], gc[q][:], AF.Sigmoid, scale=1.702, deps=[t1])
            t3 = cx.dve.op("tensor_scalar", u1[q][:], pu[q][:], bu_[:, m:m + 1], 7.0, ALU.add, ALU.min, deps=[tm])
            pu_free[q] = t3
            t4 = cx.dve.op("tensor_scalar", u1[q][:], u1[q][:], -7.0, 1.0, ALU.max, ALU.add, deps=[t3])
            t5 = cx.pool.op("tensor_tensor", gc[q][:], gc[q][:], sg[q][:], ALU.mult, deps=[t2])
            t_a = cx.dve.op("tensor_tensor", a[:, m, :], gc[q][:], u1[q][:], ALU.mult, deps=[t5, t4, a_free])
            tmp_free[q] = t_a
        xT_free = tm_last
        tm = None
        for r in range(4):
            yb = ysb[r % 2]
            for n in range(2):
                q = n
                for k in range(NCH):
                    tm = cx.pe.op("matmul", pd[q][:], a[:, k, r * 128:(r + 1) * 128], wd_[:, k, n * 512:(n + 1) * 512],
                                  start=(k == 0), stop=(k == NCH - 1),
                                  deps=[t_a, pd_free[q]] if k == 0 else (), inc=(k == NCH - 1))
                te = cx.act.op("activation", yb[:, n * 512:(n + 1) * 512], pd[q][:], AF.Copy, scale=ri_[:, r, 1:2],
                               deps=[tm, t_ri] + y_free[r % 2])
                pd_free[q] = te
            ts = cx.sp.dma(yrows[j * BLK + r * 128: j * BLK + (r + 1) * 128, :], yb[:], ysem[r % 2], deps=[te])
            y_free[r % 2] = [ts]
            yst.append(ts)
        a_free = tm
        w_free[s_] = [tm]
        ri_free[s_] = [te]
    dtoks = []
    if dbg is not None:
        ds_ = cx.dsem("dbg2")
        cx.sp.wait(yst[-2:])
        cx.sp.dma(dbg["d_yrows"], yrows, ds_)
        dtoks = [Tok(ds_.sem, ds_.cnt, ds_.key)]
    cx.end_phase(yst[-2:] + dtoks)
    if stop == "E":
        mes.__exit__(None, None, None)
        return

    cx.begin_phase("u%d" % l)
    bd = cx.sb("ubd", [NE, D], F32)
    cl = cx.dsem("uc")
    t_bd = cx.sp.dma(bd[:], A["moe_b_down%d" % l], cl)
    ygs = [cx.sb("uyg%d" % i, [128, 4, D], F32) for i in range(2)]
    ygsem = [cx.dsem("uyg%d" % i) for i in range(2)]
    yg_free = [[], []]
    xs = [cx.sb("ux%d" % i, [128, D], F32) for i in range(2)]
    xsem = [cx.dsem("ux%d" % i) for i in range(2)]
    x_free = [[], []]
    PdT = cx.sb("uPdT", [NE, 128], BF)
    Pdb = cx.sb("uPdb", [128, NE], BF)
    bdb = cx.sb("ubdb", [NE, D], BF)
    t_bd = cx.dve.op("tensor_copy", bdb[:], bd[:], deps=[t_bd])
    acc = [cx.sb("uacc%d" % i, [128, D], F32) for i in range(2)]
    osem = [cx.dsem("uo%d" % i) for i in range(2)]
    o_free = [[], []]
    pPT = cx.ps("u_pPT", [NE, 128], BF)
    pbias = [cx.ps("u_pb%d" % i, [128, 512], F32) for i in range(2)]
    pPT_free = None
    pb_free = [None, None]
    PdT_free = None
    pre = {}

    def prefetch_u(i):
        s_ = i % 2
        cx.pool.wait(yg_free[s_])
        for k in range(4):
            ins = cx.pool.e.indirect_dma_start(out=ygs[s_][:, k, :], out_offset=None, in_=yrows,
                                               in_offset=bass.IndirectOffsetOnAxis(ap=didx[:, i, k:k + 1], axis=0),
                                               bounds_check=cx.breg(R - 1), oob_is_err=False)
            ygsem[s_].cnt += 16
            ins.then_inc(ygsem[s_].sem, 16)
        t_yg = Tok(ygsem[s_].sem, ygsem[s_].cnt, ygsem[s_].key)
        t_x = cx.sp.dma(xs[s_][:], xio[i * 128:(i + 1) * 128, :], xsem[s_], deps=x_free[s_])
        pre[i] = (t_yg, t_x)

    prefetch_u(0)
    st = []
    for i in range(NT):
        if i + 1 < NT:
            prefetch_u(i + 1)
        s_ = i % 2
        t_yg, t_x = pre.pop(i)
        yg, xt, ac = ygs[s_], xs[s_], acc[s_]
        tpb = cx.dve.op("tensor_copy", Pdb[:], Pd[:, i, :], deps=[pPT_free])
        tm = cx.pe.op("transpose", pPT[:], Pdb[:], g.ident_bf[:], deps=[tpb, pPT_free])
        tc = cx.dve.op("tensor_copy", PdT[:], pPT[:], deps=[tm, PdT_free])
        pPT_free = tc
        tms = []
        for n in range(2):
            tms.append(cx.pe.op("matmul", pbias[n][:], PdT[:], bdb[:, n * 512:(n + 1) * 512], start=True, stop=True,
                                deps=[tc, t_bd, pb_free[n]]))
        PdT_free = tms[-1]
        t1 = cx.dve.op("tensor_tensor", ac[:], yg[:, 0, :], yg[:, 1, :], ALU.add, deps=[t_yg] + o_free[s_])
        t2 = cx.pool.op("tensor_tensor", yg[:, 2, :], yg[:, 2, :], yg[:, 3, :], ALU.add, deps=[t_yg])
        t3 = cx.dve.op("tensor_tensor", ac[:], ac[:], yg[:, 2, :], ALU.add, deps=[t1, t2])
        yg_free[s_] = [t3]
        for n in range(2):
            t3 = cx.dve.op("tensor_tensor", ac[:, n * 512:(n + 1) * 512], ac[:, n * 512:(n + 1) * 512], pbias[n][:], ALU.add,
                           deps=[t3, tms[n]])
            pb_free[n] = t3
        t4 = cx.dve.op("tensor_tensor", ac[:], ac[:], g.gb[:, 1 + 2 * l, :], ALU.mult, deps=[t3])
        t5 = cx.dve.op("tensor_tensor", ac[:], ac[:], xt[:], ALU.add, deps=[t4, t_x])
        x_free[s_] = [t5]
        ts = cx.sp.dma(xio[i * 128:(i + 1) * 128, :], ac[:], osem[s_], deps=[t5])
        o_free[s_] = [ts]
        st.append(ts)
    cx.end_phase(st[-2:])
    mes.__exit__(None, None, None)


def emit_attn(cx, g, A, S, xio, dbg=None, stop=None):
    nc = cx.nc
    NT = S // 128
    NB = S // 256
    NJ = S // 512
    HD = 64
    NH = 16
    kT_scr = nc.dram_tensor("kT_scr", [NH * 64, S], BF, kind="Internal").ap()
    qT_scr = nc.dram_tensor("qT_scr", [NH * 64, S], BF, kind="Internal").ap()
    aT_scr = nc.dram_tensor("aT_scr", [NH * 32, S], BF, kind="Internal").ap()
    aes = ExitStack()
    aes.__enter__()
    vaug = aes.enter_context(nc.sbuf_tensor("sb_vaug", [128, NT, NH, 65], BF))

    cx.begin_phase("q")
    w_kv = cx.sb("w_kv", [128, NCH, 2 * D], BF)
    w_q = cx.sb("w_q", [128, NCH, D], BF)
    kgb = cx.sb("kgb", [128, D], F32)
    qgb = cx.sb("qgb", [128, D], F32)
    qaugc = cx.sb("qaugc", [128, 4, NH, 6], F32)
    wl = cx.dsem("qw")
    for c in range(NCH):
        cx.pool.dma(w_kv[:, c, :], A["w_kv"][c * 128:(c + 1) * 128, :], wl, max_dma_last_dim=4096)
        cx.pool.dma(w_q[:, c, :], A["attn_w_q"][c * 128:(c + 1) * 128, :], wl, max_dma_last_dim=4096)
    cx.sp.dma(kgb[:], A["kg_t"].partition_broadcast(128), wl)
    cx.sp.dma(qgb[:], A["qg_t"].partition_broadcast(128), wl)
    cx.sp.dma(qaugc[:], A["qaug_c"], wl)
    t_w = Tok(wl.sem, wl.cnt, wl.key)
    t_qg = cx.dve.op("tensor_scalar", qgb[:], qgb[:], 0.125, None, ALU.mult, deps=[t_w])
    t_v1 = cx.dve.op("memset", vaug[:], 1.0)

    xr = Ring([cx.sb("qx%d" % i, [128, D], F32) for i in range(2)])
    xsem = [cx.dsem("qx%d" % i) for i in range(2)]
    work = dict(junk=cx.sb("qjunk", [128, D], BF), ssq=cx.sb("qssq", [128, 1], F32),
                rstd=cx.sb("qrstd", [128, 1], F32), xn=cx.sb("qxn", [128, D], BF))
    h1T = cx.sb("qh1T", [128, NCH, 128], BF)
    hkT = cx.sb("qhkT", [128, NCH, 128], BF)
    ssq16 = cx.sb("qssq16", [128, 2, NH], F32)
    tmpn = cx.sb("qtmpn", [128, D], F32)
    sq = tmpn
    kn = cx.sb("qkn", [128, D], BF)
    qn = cx.sb("qqn", [128, D], BF)
    KT4 = cx.sb("qKT4", [128, NCH, 256], BF)
    QT4 = cx.sb("qQT4", [128, NCH, 256], BF)
    AT4 = cx.sb("qAT4", [128, 4, 256], BF)
    QTt = cx.sb("qQTt", [128, NCH, 128], BF)
    ksum = cx.sb("qksum", [128, NCH, NT], F32)
    kmT = cx.sb("qkmT", [128, NCH, 2, NB], BF)
    t_kz = cx.dve.op("memset", kmT[:], 0.0)
    gm = cx.sb("qgm", [128, NH, 16], F32)
    m8h = cx.sb("qm8h", [128, NH, 8], F32)
    augt = cx.sb("qaugt", [128, NH, 32], BF)
    stsem = cx.dsem("qst")
    pT = cx.ps("q_pT", [128, NCH, 128], BF)
    pk = cx.ps("q_pk", [128, D], F32)
    pvq = cx.ps("q_pvq", [128, D], F32)
    ptr = cx.ps("q_ptr", [128, NCH, 128], BF)
    pkm = cx.ps("q_pkm", [128, 16], F32)
    pkm_free = None
    pgt = cx.ps("q_pgt", [128, NH, 16], F32)
    t_az = cx.dve.op("memset", augt[:], 0.0)
    t_gz = cx.dve.op("memset", gm[:], -1e30, deps=[t_az])
    pT_free = pk_free = pvq_free = ptr_free = pat_free = pgt_free = None
    t_km = None
    hT_free = None
    st4_free = []
    loads = {}

    def load(i):
        buf, fr, si = xr.next()
        loads[i] = (buf, si, cx.sp.dma(buf[:], xio[i * 128:(i + 1) * 128, :], xsem[si], deps=fr))

    import os
    NTQ = int(os.environ.get("DBG_NTQ", NT))
    QS = int(os.environ.get("DBG_QS", 99))
    load(0)
    st = []
    for i in range(NTQ):
        if i + 1 < NTQ:
            load(i + 1)
        own = i // 2
        i4 = i % 4
        i2 = i % 2
        xt, xsi, tl = loads.pop(i)
        junk, ssq, rstd, xn = work["junk"], work["ssq"], work["rstd"], work["xn"]
        t = cx.act.op("activation", junk[:], xt[:], AF.Square, accum_out=ssq[:], deps=[tl])
        t = emit_rstd(cx, ssq[:], rstd[:], D, [t])
        t_xn = cx.act.op("activation", xn[:], xt[:], AF.Copy, scale=rstd[:], deps=[t])
        xr.release(xsi, t_xn)
        tm = None
        for c in range(NCH):
            tm = cx.pe.op("transpose", pT[:, c, :], xn[:, c * 128:(c + 1) * 128], g.ident_bf[:],
                          deps=[t_xn, pT_free] if c == 0 else (), inc=(c == NCH - 1))
        te1 = te2 = None
        for c in range(NCH):
            te1 = cx.dve.op("tensor_scalar", h1T[:, c, :], pT[:, c, :], g.gsc[:, 2, c:c + 1], g.shc[:, 2, c:c + 1],
                            ALU.mult, ALU.add, deps=[tm, hT_free] if c == 0 else ())
            te2 = cx.dve.op("tensor_scalar", hkT[:, c, :], pT[:, c, :], g.gsc[:, 4, c:c + 1], g.shc[:, 4, c:c + 1],
                            ALU.mult, ALU.add, deps=[tm, hT_free] if c == 0 else ())
        pT_free = [te1, te2]
        if QS < 1:
            continue
        tmk = None
        for n in range(2):
            for k in range(NCH):
                tmk = cx.pe.op("matmul", pk[:, n * 512:(n + 1) * 512], hkT[:, k, :], w_kv[:, k, n * 512:(n + 1) * 512],
                               start=(k == 0), stop=(k == NCH - 1), deps=[te2, t_w, pk_free] if (k == 0 and n == 0) else (),
                               inc=(k == NCH - 1 and n == 1))
        tmv = None
        for n in range(2):
            for k in range(NCH):
                tmv = cx.pe.op("matmul", pvq[:, n * 512:(n + 1) * 512], hkT[:, k, :],
                               w_kv[:, k, D + n * 512: D + (n + 1) * 512], start=(k == 0), stop=(k == NCH - 1),
                               deps=[pvq_free] if (k == 0 and n == 0) else (), inc=(k == NCH - 1 and n == 1))
        if QS < 2:
            continue
        t = cx.act.op("activation", sq[:], pk[:], AF.Square, deps=[tmk])
        t = cx.dve.op("tensor_reduce", ssq16[:, 0, :], sq[:].rearrange("p (h d) -> p h d", d=HD), AX.X, ALU.add, deps=[t])
        t = emit_rstd(cx, ssq16[:, 0, :], ssq16[:, 0, :], HD, [t])
        t = cx.dve.op("tensor_tensor", tmpn[:].rearrange("p (h d) -> p h d", d=HD), pk[:].rearrange("p (h d) -> p h d", d=HD),
                      ssq16[:, 0, :].unsqueeze(2).to_broadcast([128, NH, HD]), ALU.mult, deps=[t])
        pk_free = t
        t_kn = cx.dve.op("tensor_tensor", kn[:], tmpn[:], kgb[:], ALU.mult, deps=[t, t_w, ptr_free])
        if QS < 3:
            continue
        t_v = cx.dve.op("tensor_copy", vaug[:, i, :, 0:HD], pvq[:].rearrange("p (h d) -> p h d", d=HD),
                        deps=[tmv, t_v1])
        if QS < 4:
            continue
        tmq = None
        for n in range(2):
            for k in range(NCH):
                qv = os.environ.get("DBG_QVAR", "")
                tmq = cx.pe.op("matmul", pvq[:, n * 512:(n + 1) * 512], (hkT if "h" in qv else h1T)[:, k, :],
                               (w_kv if "w" in qv else w_q)[:, k, n * 512:(n + 1) * 512],
                               start=(k == 0), stop=(k == NCH - 1), deps=[te1, t_v] if (k == 0 and n == 0) else (),
                               inc=(k == NCH - 1 and n == 1))
        hT_free = tmq
        if QS < 5:
            continue
        tmt = None
        for h in range(NCH):
            tmt = cx.pe.op("transpose", ptr[:, h, :], kn[:, h * 128:(h + 1) * 128], g.ident_bf[:],
                           deps=[t_kn, ptr_free] if h == 0 else (), inc=(h == NCH - 1))
        K5 = int(os.environ.get("DBG_K5", 3))
        t_k4 = t_ks = None
        if K5 & 1:
            t_k4 = cx.act.op("activation", KT4[:, :, i2 * 128:(i2 + 1) * 128], ptr[:], AF.Copy, deps=[tmt] + st4_free)
        if K5 & 2:
            tmk2 = None
            for c in range(NCH):
                tmk2 = cx.pe.op("matmul", pkm[:, c:c + 1], kn[:, c * 128:(c + 1) * 128], g.ones_bf[:, 0:1], start=True, stop=True,
                                deps=[pkm_free] if c == 0 else (), inc=(c == NCH - 1))
            t_ks = cx.dve.op("tensor_copy", ksum[:, :, i], pkm[:, 0:NCH], deps=[tmk2])
            pkm_free = t_ks
        ptr_free = [t_k4, t_ks]
        if K5 != 3:
            continue
        if i % 2 == 1:
            t = cx.dve.op("tensor_tensor", ksum[:, :, i], ksum[:, :, i], ksum[:, :, i - 1], ALU.add, deps=[t_ks])
            cx.dve.op("tensor_scalar", kmT[0:64, :, 0, own], ksum[0:64, :, i], 1.0 / 256.0, None, ALU.mult, deps=[t, t_kz])
            t_km = cx.dve.op("tensor_scalar", kmT[64:128, :, 1, own], ksum[64:128, :, i], 1.0 / 256.0, None, ALU.mult)
        if QS < 6:
            continue
        t = cx.act.op("activation", sq[:], pvq[:], AF.Square, deps=[tmq])
        t = cx.dve.op("tensor_reduce", ssq16[:, 1, :], sq[:].rearrange("p (h d) -> p h d", d=HD), AX.X, ALU.add, deps=[t])
        t = emit_rstd(cx, ssq16[:, 1, :], ssq16[:, 1, :], HD, [t])
        t = cx.dve.op("tensor_tensor", tmpn[:].rearrange("p (h d) -> p h d", d=HD), pvq[:].rearrange("p (h d) -> p h d", d=HD),
                      ssq16[:, 1, :].unsqueeze(2).to_broadcast([128, NH, HD]), ALU.mult, deps=[t])
        pvq_free = t
        t_qn = cx.dve.op("tensor_tensor", qn[:], tmpn[:], qgb[:], ALU.mult, deps=[t, t_qg])
        K6 = int(os.environ.get("DBG_K6", 9))
        if K6 < 2:
            continue
        tmt = None
        for h in range(NCH):
            tmt = cx.pe.op("transpose", ptr[:, h, :], qn[:, h * 128:(h + 1) * 128], g.ident_bf[:],
                           deps=[t_qn, ptr_free] if h == 0 else (), inc=(h == NCH - 1))
        t_q4 = cx.act.op("activation", QT4[:, :, i2 * 128:(i2 + 1) * 128], ptr[:], AF.Copy, deps=[tmt] + st4_free)
        if K6 < 3:
            ptr_free = [t_q4]
            continue
        t_qt = t_q4
        ptr_free = [t_q4, t_qt]
        if QS < 7:
            continue
        if own > 0:
            tmg = None
            for c in range(NCH):
                tmg = cx.pe.op("matmul", pgt[:, 2 * c:2 * c + 2, 0:own], QT4[:, c, i2 * 128:(i2 + 1) * 128], kmT[:, c, :, 0:own],
                               start=True, stop=True,
                               deps=[t_qt, t_km, pgt_free] if c == 0 else (), inc=(c == NCH - 1))
            t = cx.dve.op("tensor_copy", gm[:, :, 0:own], pgt[:, :, 0:own], deps=[tmg, t_gz])
            pgt_free = t
            for h in range(NH):
                t = cx.dve.op("max", m8h[:, h, :], gm[:, h, :], deps=[t] if h == 0 else ())
            for h in range(NH):
                t = cx.dve.op("tensor_scalar", augt[:, h, 0:16], gm[:, h, :], m8h[:, h, 2:3], NEG, ALU.is_lt, ALU.mult,
                              deps=[t, pat_free] if h == 0 else ())
            t = cx.dve.op("memset", augt[:, :, own:own + 1], 0.0, deps=[t])
        else:
            t = None
        t_au = cx.dve.op("tensor_copy", augt[:, :, 16:22], qaugc[:, i4, :, :], deps=[t, t_w, pat_free, t_az])
        tma = None
        for h in range(4):
            tma = cx.pe.op("transpose", ptr[:, h, :], augt[:, 4 * h:4 * h + 4, :].rearrange("p a b -> p (a b)"), g.ident_bf[:],
                           deps=[t_au, ptr_free] if h == 0 else (), inc=(h == 3))
        t_a4 = cx.act.op("activation", AT4[:, :, i2 * 128:(i2 + 1) * 128], ptr[:, 0:4, :], AF.Copy, deps=[tma] + st4_free)
        pat_free = t_a4
        ptr_free = [t_a4]
        if QS < 8:
            continue
        if i2 == 1:
            j0 = (i - 1) * 128
            cx.sp.dma(kT_scr.rearrange("(c p) s -> p c s", p=128)[:, :, j0:j0 + 256], KT4[:], stsem, deps=[t_k4, t_q4, t_a4])
            cx.sp.dma(qT_scr.rearrange("(c p) s -> p c s", p=128)[:, :, j0:j0 + 256], QT4[:], stsem)
            ts = cx.sp.dma(aT_scr.rearrange("(c p) s -> p c s", p=128)[:, :, j0:j0 + 256], AT4[:], stsem)
            st4_free = [ts]
            st.append(ts)
    dtoks = []
    if dbg is not None and QS >= 99:
        ds_ = cx.dsem("dbgq")
        cx.sp.wait(st[-1:])
        cx.sp.dma(dbg["d_kT"], kT_scr, ds_)
        cx.sp.dma(dbg["d_qT"], qT_scr, ds_)
        cx.sp.dma(dbg["d_aT"], aT_scr, ds_)
        cx.sp.dma(dbg["d_v"], vaug[:], ds_, deps=[t_v])
        dtoks = [Tok(ds_.sem, ds_.cnt, ds_.key)]
    cx.end_phase((st[-1:] + [t_v] + dtoks) if QS >= 99 else [])
    if stop == "Q":
        aes.__exit__(None, None, None)
        return

    cx.begin_phase("a")
    cmask = cx.sb("cmask", [128, 4, 512], BF)
    cbias = cx.sb("cbias", [128, NH, 32], F32)
    otok = cx.sb("otok", [128, NT, D], BF)
    wl = cx.dsem("aw")
    cx.pool.dma(cmask[:], A["cmask"], wl, max_dma_last_dim=4096)
    cx.sp.dma(cbias[:], A["cbias"], wl)
    t_w = Tok(wl.sem, wl.cnt, wl.key)
    hsem = [cx.dsem("ah%d" % i) for i in range(2)]
    h_free = [[], []]
    PTs = [cx.sb("aPT%d" % i, [128, 512], BF) for i in range(3)]
    PT_free = [None] * 3
    OTs = cx.sb("aOT", [65, 512], BF)
    Sd = [cx.sb("aSd%d" % i, [128, 512], F32) for i in range(2)]
    Sd_free = [None, None]
    ndiag = 0
    rec = cx.sb("arec", [128, 1], F32)
    sub = ExitStack()
    KTs = [sub.enter_context(nc.sbuf_tensor("sb_aKT%d" % i, [96, S], BF)) for i in range(2)]
    QTs = [sub.enter_context(nc.sbuf_tensor("sb_aQT%d" % i, [96, S], BF)) for i in range(2)]
    pS = [cx.ps("a_pS%d" % i, [128, 512], F32) for i in range(2)]
    pO = [cx.ps("a_pO%d" % i, [65, 512], F32) for i in range(2)]
    pOt = [cx.ps("a_pOt%d" % i, [128, 128], BF) for i in range(2)]
    pS_free = [None, None]
    pO_free = [None, None]
    pOt_free = [None, None]
    OT_free = None
    pre = {}

    def prefetch_h(h):
        s_ = h % 2
        cx.sp.dma(KTs[s_][0:64, :], kT_scr[h * 64:(h + 1) * 64, :], hsem[s_], deps=h_free[s_])
        cx.sp.dma(QTs[s_][64:96, :], aT_scr[h * 32:(h + 1) * 32, :], hsem[s_])
        cx.pool.dma(KTs[s_][64:96, :], A["kaug_c"][h], hsem[s_], deps=h_free[s_], max_dma_last_dim=4096)
        pre[h] = cx.sp.dma(QTs[s_][0:64, :], qT_scr[h * 64:(h + 1) * 64, :], hsem[s_])

    prefetch_h(0)
    npair = 0
    nj = 0
    t_last = None
    import os
    NHL = int(os.environ.get("DBG_NH", NH))
    NJL = int(os.environ.get("DBG_NJ", NJ))
    for h in range(NHL):
        if h + 1 < NHL:
            prefetch_h(h + 1)
        s_ = h % 2
        KT, QT = KTs[s_], QTs[s_]
        t_h = pre.pop(h)
        tmo = None
        for j in range(NJL):
            po = pO[nj % 2]
            ni = 4 * j + 4
            tms_of = {}

            def s_mm(ii):
                n_ = npair + ii
                tms_of[ii] = cx.pe.op("matmul", pS[n_ % 2][:], KT[:, ii * 128:(ii + 1) * 128], QT[:, j * 512:(j + 1) * 512],
                                      start=True, stop=True, deps=[t_h, pS_free[n_ % 2]])

            s_mm(0)
            for i in range(ni):
                n_ = npair + i
                ps_ = pS[n_ % 2]
                PT = PTs[n_ % 3]
                tms = tms_of.pop(i)
                dd = 4 * j - i + 3
                if i >= 4 * j:
                    sd = Sd[ndiag % 2]
                    td = cx.dve.op("tensor_tensor", sd[:], ps_[:], cmask[:, i - 4 * j, :], ALU.add,
                                   deps=[tms, t_w, Sd_free[ndiag % 2]])
                    pS_free[n_ % 2] = td
                    te = cx.act.op("activation", PT[:], sd[:], AF.Exp, bias=cbias[:, h, dd:dd + 1],
                                   deps=[td, PT_free[n_ % 3]])
                    Sd_free[ndiag % 2] = te
                    ndiag += 1
                else:
                    te = cx.act.op("activation", PT[:], ps_[:], AF.Exp, bias=cbias[:, h, dd:dd + 1],
                                   deps=[tms, t_w, PT_free[n_ % 3]])
                    pS_free[n_ % 2] = te
                if i + 1 < ni:
                    s_mm(i + 1)
                tmo = cx.pe.op("matmul", po[:], vaug[:, i, h, :], PT[:], start=(i == 0), stop=(i == ni - 1),
                               deps=[te, pO_free[nj % 2]] if i == 0 else [te])
                PT_free[n_ % 3] = tmo
            npair += ni
            t_ot = cx.dve.op("tensor_copy", OTs[:], po[:], deps=[tmo, OT_free])
            pO_free[nj % 2] = t_ot
            tmt = None
            for r in range(4):
                pt_ = pOt[r % 2]
                tmt = cx.pe.op("transpose", pt_[:, 0:65], OTs[0:65, r * 128:(r + 1) * 128], g.ident_bf[0:65, 0:65],
                               deps=[t_ot, pOt_free[r % 2]])
                t1 = cx.dve.op("reciprocal", rec[:], pt_[:, 64:65], deps=[tmt])
                t_last = cx.dve.op("tensor_scalar", otok[:, 4 * j + r, h * HD:(h + 1) * HD], pt_[:, 0:64], rec[:, 0:1], None,
                                   ALU.mult, deps=[t1])
                pOt_free[r % 2] = t_last
            OT_free = tmt
            nj += 1
        h_free[s_] = [tmo]
    for e_ in cx.engs:
        e_.wait(tmo, t_last)
    sub.close()
    w_o = cx.sb("w_o", [128, NCH, D], BF)
    wl2 = cx.dsem("aw2")
    for c in range(NCH):
        cx.pool.dma(w_o[:, c, :], A["attn_w_o"][c * 128:(c + 1) * 128, :], wl2, max_dma_last_dim=4096)
    t_w = Tok(wl2.sem, wl2.cnt, wl2.key)
    xr = [cx.sb("ax%d" % i, [128, D], F32) for i in range(2)]
    xsem = [cx.dsem("ax%d" % i) for i in range(2)]
    x_free = [[], []]
    oT = cx.sb("aoT", [128, NCH, 128], BF)
    ob = [cx.sb("aob%d" % i, [128, D], F32) for i in range(2)]
    osem = [cx.dsem("ao%d" % i) for i in range(2)]
    o_free = [[], []]
    poT = cx.ps("a_poT", [128, NCH, 128], BF)
    oT_free = None
    poT_free = None
    pre = {}

    def load_x(i):
        s_ = i % 2
        pre[i] = cx.sp.dma(xr[s_][:], xio[i * 128:(i + 1) * 128, :], xsem[s_], deps=x_free[s_])

    load_x(0)
    st = []
    for i in range(NT):
        if i + 1 < NT:
            load_x(i + 1)
        s_ = i % 2
        t_x = pre.pop(i)
        tm = None
        for c in range(NCH):
            tm = cx.pe.op("transpose", poT[:, c, :], otok[:, i, c * 128:(c + 1) * 128], g.ident_bf[:],
                          deps=[t_last, poT_free] if c == 0 else (), inc=(c == NCH - 1))
        te = cx.act.op("activation", oT[:], poT[:], AF.Copy, deps=[tm, oT_free])
        poT_free = te
        t1 = None
        for n in range(2):
            p = pS[n]
            for k in range(NCH):
                tm = cx.pe.op("matmul", p[:], oT[:, k, :], w_o[:, k, n * 512:(n + 1) * 512], start=(k == 0), stop=(k == NCH - 1),
                              deps=[te, t_w, pS_free[n]] if k == 0 else (), inc=(k == NCH - 1))
            t1 = cx.dve.op("tensor_tensor", ob[s_][:, n * 512:(n + 1) * 512], p[:], g.gb[:, 2, n * 512:(n + 1) * 512], ALU.mult,
                           deps=[tm] + o_free[s_])
            pS_free[n] = t1
        oT_free = tm
        t2 = cx.dve.op("tensor_tensor", ob[s_][:], ob[s_][:], xr[s_][:], ALU.add, deps=[t1, t_x])
        x_free[s_] = [t2]
        ts = cx.sp.dma(xio[i * 128:(i + 1) * 128, :], ob[s_][:], osem[s_], deps=[t2])
        o_free[s_] = [ts]
        st.append(ts)
    cx.end_phase(st[-2:])
    aes.__exit__(None, None, None)


def build_program(S, phases=("sgu",), dbg=False, stop=None):
    _, _, rec = _build_program(S, phases, dbg, stop, None)
    nc, names, _ = _build_program(S, phases, dbg, stop, rec)
    return nc, names


def _build_program(S, phases, dbg, stop, needed):
    nc = bass.Bass("TRN2", target_bir_lowering=False)
    A = {}

    def inp(name, shape, dt=F32):
        A[name] = nc.dram_tensor(name, list(shape), dt, kind="ExternalInput").ap()

    inp("x", [S, D])
    inp("c_col", [128, NCH])
    inp("ident", [128, 128])
    inp("ngc", [128, 5, NCH])
    inp("ada_w", [2, D, 6 * D])
    inp("ada_b", [2, 6 * D])
    inp("kv_ada_w", [D, 2 * D])
    inp("kv_ada_b", [1, 2 * D])
    inp("sgu_w_in", [D, 2 * DS])
    inp("sgu_b_in", [1, 2 * DS])
    inp("sgu_v_g", [DS])
    inp("sgu_w_sT", [128, 8, 128])
    inp("trimask", [128, 128])
    inp("sgu_b_sT", [128, 8])
    inp("sgu_w_out", [DS, D])
    for l_ in range(2):
        if ("moe%d" % l_) not in phases:
            continue
        inp("moe_w_router%d" % l_, [D, NE])
        inp("moe_b_router%d" % l_, [1, NE])
        inp("moe_w_gate%d" % l_, [NE, D, D])
        inp("moe_w_up%d" % l_, [NE, D, D])
        inp("moe_w_down%d" % l_, [NE, D, D])
        inp("moe_b_gate%d" % l_, [NE, D])
        inp("moe_b_up%d" % l_, [NE, D])
        inp("moe_b_down%d" % l_, [NE, D])
    if "attn" in phases:
        inp("w_kv", [D, 2 * D])
        inp("attn_w_q", [D, D])
        inp("attn_w_o", [D, D])
        inp("kg_t", [D])
        inp("qg_t", [D])
        inp("qaug_c", [128, 4, 16, 6])
        inp("kaug_c", [16, 32, S])
        inp("cmask", [128, 4, 512])
        inp("cbias", [128, 16, 32])
    inp("lstrict", [128, 128])
    inp("tokid", [128, 32])
    inp("pcol", [128, 1])
    inp("jgridb", [128, 64, NE])
    y = nc.dram_tensor("y", [S, D], F32, kind="ExternalOutput").ap()
    cx = Ctx(nc, needed)
    with cx.es:
        g = emit_globals(cx, A)
        if "sgu" in phases:
            emit_sgu(cx, g, A, S, A["x"], y)
        if "moe0" in phases:
            dd = None
            if dbg:
                NT_ = S // 128
                NB_ = (S * 4) // BLK + NE
                dd = {}
                for nm, shp, dt in (("d_didx", [128, NT_, 4], I32), ("d_idxw", [128, NB_], I32), ("d_Pd", [128, NT_, NE], F32),
                                    ("d_rowinfo", [NB_ * BLK, 2], F32), ("d_cnt", [128, NE], F32), ("d_yrows", [NB_ * BLK, D], F32), ("d_h2", [S, D], BF)):
                    dd[nm] = nc.dram_tensor(nm, shp, dt, kind="ExternalOutput").ap()
            emit_moe(cx, g, A, S, 0, y, dbg=dd, stop=stop)
        if "copy" in phases:
            cs_ = cx.dsem("cpy")
            tcp = None
            for i_ in range(S // 128):
                tcp = cx.sp.dma(y[i_ * 128:(i_ + 1) * 128, :], A["x"][i_ * 128:(i_ + 1) * 128, :], cs_)
            for e_ in cx.engs:
                e_.wait(tcp)
            nc.all_engine_barrier()
        if "attn" in phases:
            dd = None
            if dbg:
                dd = {}
                for nm, shp, dt in (("d_kT", [1024, S], BF), ("d_qT", [1024, S], BF), ("d_aT", [512, S], BF), ("d_v", [128, S // 128, 16, 65], BF)):
                    dd[nm] = nc.dram_tensor(nm, shp, dt, kind="ExternalOutput").ap()
            emit_attn(cx, g, A, S, y, dbg=dd, stop=stop)
        if "moe1" in phases:
            emit_moe(cx, g, A, S, 1, y)
    return nc, list(A.keys()), cx.rec


def host_inputs(inputs, b, S):
    f = np.float32
    c = inputs["c"][b]
    d = {}
    d["x"] = np.ascontiguousarray(inputs["x"][b, :S])
    d["c_col"] = np.ascontiguousarray(c.reshape(NCH, 128).T)
    d["ident"] = np.eye(128, dtype=f)
    gains = [inputs["norm1_g"][0], inputs["norm1_g"][1], inputs["norm2_g"][0], inputs["norm2_g"][1], inputs["kv_norm_g"]]
    d["ngc"] = np.ascontiguousarray(np.stack([gv.reshape(NCH, 128).T for gv in gains], axis=1))
    d["ada_w"] = inputs["ada_w"]
    d["ada_b"] = inputs["ada_b"]
    d["kv_ada_w"] = inputs["kv_ada_w"]
    d["kv_ada_b"] = inputs["kv_ada_b"].reshape(1, -1)
    d["sgu_w_in"] = inputs["sgu_w_in"][0]
    d["sgu_b_in"] = inputs["sgu_b_in"][0].reshape(1, -1)
    d["sgu_v_g"] = inputs["sgu_v_g"][0]
    d["sgu_w_sT"] = np.ascontiguousarray(inputs["sgu_w_s"][0].transpose(2, 0, 1))
    d["trimask"] = np.triu(np.ones((128, 128), f))
    d["sgu_b_sT"] = np.ascontiguousarray(inputs["sgu_b_s"][0].T)
    d["sgu_w_out"] = inputs["sgu_w_out"][0]
    for l_ in range(2):
        for k in ("moe_w_router", "moe_w_gate", "moe_w_up", "moe_w_down", "moe_b_gate", "moe_b_up", "moe_b_down"):
            d["%s%d" % (k, l_)] = inputs[k][l_]
        d["moe_b_router%d" % l_] = inputs["moe_b_router"][l_].reshape(1, NE)
    import ml_dtypes
    bf = ml_dtypes.bfloat16
    d["w_kv"] = inputs["w_kv"]
    d["attn_w_q"] = inputs["attn_w_q"][0]
    d["attn_w_o"] = inputs["attn_w_o"][0]
    d["kg_t"] = np.tile(inputs["k_norm_g"], 16).astype(f)
    d["qg_t"] = np.tile(inputs["q_norm_g"][0], 16).astype(f)
    slopes = (2.0 ** (-8.0 * np.arange(1, 17, dtype=np.float64) / 16)).astype(f)
    s_hi = slopes.astype(bf).astype(f)
    s_lo = (slopes - s_hi).astype(bf).astype(f)
    qi = np.arange(512)
    Aq = (16 * (qi // 16)).astype(f).reshape(4, 128).T
    Bq = (qi % 16).astype(f).reshape(4, 128).T
    qa = np.zeros((128, 4, 16, 6), f)
    qa[:, :, :, 0] = -Aq[:, :, None]
    qa[:, :, :, 1] = -Bq[:, :, None]
    qa[:, :, :, 2] = -Aq[:, :, None]
    qa[:, :, :, 3] = -Bq[:, :, None]
    qa[:, :, :, 4] = s_hi[None, None, :]
    qa[:, :, :, 5] = s_lo[None, None, :]
    d["qaug_c"] = qa
    pos = np.arange(S)
    ka = np.zeros((16, 32, S), f)
    ka[:, :16, :] = (pos[None, :] // 256 == np.arange(16)[:, None]).astype(f)[None]
    ka[:, 16, :] = s_hi[:, None]
    ka[:, 17, :] = s_hi[:, None]
    ka[:, 18, :] = s_lo[:, None]
    ka[:, 19, :] = s_lo[:, None]
    ka[:, 20, :] = (pos % 128)[None, :]
    ka[:, 21, :] = (pos % 128)[None, :]
    d["kaug_c"] = ka
    ki = np.arange(128)
    cm = np.zeros((128, 4, 512), f)
    for dd in range(4):
        cm[:, dd, :] = np.where(128 * dd + ki[:, None] <= qi[None, :], 0.0, NEG)
    d["cmask"] = cm
    cb = np.zeros((128, 16, 32), f)
    cb[:] = (-slopes[:, None].astype(np.float64) * 128.0 * (np.arange(32)[None, :] - 3)).astype(f)[None]
    d["cbias"] = cb
    d["lstrict"] = np.triu(np.ones((128, 128), f), 1)
    d["tokid"] = (np.arange(32)[None, :] * 128 + np.arange(128)[:, None]).astype(f)
    d["pcol"] = np.arange(128, dtype=f).reshape(128, 1)
    d["jgridb"] = np.ascontiguousarray(np.broadcast_to(np.arange(64, dtype=f)[None, :, None], (128, 64, NE)))
    return d


_CACHE = {}


def kernel(**inputs):
    inputs = {k: np.asarray(v) for k, v in inputs.items()}
    B, S, _ = inputs["x"].shape
    nc, names = build_program(S, phases=("sgu", "moe0", "attn", "moe1"))
    in_maps = []
    for b in range(B):
        d = host_inputs(inputs, b, S)
        in_maps.append({k: d[k] for k in names})
    res = run_bass_kernel_spmd(nc, in_maps, core_ids=list(range(B)))
    return np.stack([r["y"] for r in res.results], axis=0)
```
